# Optimizing a Trainium2 kernel written in Bass

```python
import math
import jax, jax.numpy as jnp
from jax import lax
import numpy as np

D_MODEL = 1024
BATCH = 8
SEQ = 4096
DEPTH = 1

ATTN_HEADS = 8
ATTN_HEAD_DIM = 64
ATTN_WIDTH = ATTN_HEADS * ATTN_HEAD_DIM
MOBA_BLOCK = 256
MOBA_TOPK = 3
Q_CHUNK = 32
POOL_WIDTH = D_MODEL // 2
POOL_WINDOWS = (2, 4, 8, 16)
POOL_GROUPS = len(POOL_WINDOWS)
POOL_GROUP_DIM = POOL_WIDTH // POOL_GROUPS
IN_WIDTH = 3 * ATTN_WIDTH + POOL_WIDTH + 2 * D_MODEL
PEER_HEADS = 8
PEER_KEYS = 128
PEER_EXPERTS = PEER_KEYS * PEER_KEYS
PEER_QUERY_DIM = 256
PEER_HALF = PEER_QUERY_DIM // 2
PEER_TOPK = 16
PEER_CHUNK = 128
RMS_EPS = 1e-6
NEG_INF = -1e30

kernel_name = "hybrid_moba_pool_peer_block"


def rms_norm(x, gain):
    xf = x.astype(jnp.float32)
    y = xf * lax.rsqrt(jnp.mean(xf * xf, axis=-1, keepdims=True) + RMS_EPS)
    return (y * gain.astype(jnp.float32)).astype(x.dtype)


def modulate(h, shift, scale):
    return h * (1.0 + scale[:, None, :]) + shift[:, None, :]


def alibi_slopes(n_heads):
    return jnp.asarray(2.0 ** (-8.0 * (np.arange(n_heads) + 1) / n_heads), dtype=jnp.float32)


def moba_attention(q, k, v):
    B, H, S, hd = q.shape
    nb = -(-S // MOBA_BLOCK)
    s_pad = nb * MOBA_BLOCK
    pad = ((0, 0), (0, 0), (0, s_pad - S), (0, 0))
    kp = jnp.pad(k, pad)
    vp = jnp.pad(v, pad)
    kb = kp.reshape(B, H, nb, MOBA_BLOCK, hd)
    vb = vp.reshape(B, H, nb, MOBA_BLOCK, hd)
    k_mean = jnp.mean(kb.astype(jnp.float32), axis=3)
    n_sel = min(MOBA_TOPK, nb - 1)
    slopes = alibi_slopes(H)[None, :, None, None]
    scale = hd ** -0.5
    b_ix = jnp.arange(B)[:, None, None, None]
    h_ix = jnp.arange(H)[None, :, None, None]
    in_block = jnp.arange(MOBA_BLOCK)

    def chunk(start):
        qc = lax.dynamic_slice_in_dim(q, start, Q_CHUNK, axis=2).astype(jnp.float32) * scale
        q_pos = start + jnp.arange(Q_CHUNK)
        blk = start // MOBA_BLOCK
        own_start = blk * MOBA_BLOCK
        k_own = lax.dynamic_slice_in_dim(kp, own_start, MOBA_BLOCK, axis=2).astype(jnp.float32)
        v_own = lax.dynamic_slice_in_dim(vp, own_start, MOBA_BLOCK, axis=2).astype(jnp.float32)
        dist_own = q_pos[:, None] - (own_start + in_block)[None, :]
        lg_own = jnp.einsum('bhqd,bhkd->bhqk', qc, k_own) - slopes * dist_own
        lg_own = jnp.where(dist_own >= 0, lg_own, NEG_INF)
        if n_sel == 0:
            p_own = jax.nn.softmax(lg_own, axis=-1)
            out = jnp.einsum('bhqk,bhkd->bhqd', p_own, v_own)
            return out.astype(q.dtype)
        gate = jnp.einsum('bhqd,bhnd->bhqn', qc, k_mean)
        gate = jnp.where(jnp.arange(nb) < blk, gate, NEG_INF)
        _, sel = lax.top_k(gate, n_sel)
        sel_valid = jnp.arange(n_sel) < blk
        k_sel = kb[b_ix, h_ix, sel].astype(jnp.float32)
        v_sel = vb[b_ix, h_ix, sel].astype(jnp.float32)
        dist_sel = q_pos[None, None, :, None, None] - (sel[..., None] * MOBA_BLOCK + in_block)
        lg_sel = jnp.einsum('bhqd,bhqnkd->bhqnk', qc, k_sel) - slopes[..., None] * dist_sel
        lg_sel = jnp.where(sel_valid[:, None], lg_sel, NEG_INF)
        lg = jnp.concatenate([lg_sel.reshape(B, H, Q_CHUNK, n_sel * MOBA_BLOCK), lg_own], axis=-1)
        p = jax.nn.softmax(lg, axis=-1)
        p_sel = p[..., :n_sel * MOBA_BLOCK].reshape(B, H, Q_CHUNK, n_sel, MOBA_BLOCK)
        p_own = p[..., n_sel * MOBA_BLOCK:]
        out = (jnp.einsum('bhqnk,bhqnkd->bhqd', p_sel, v_sel)
               + jnp.einsum('bhqk,bhkd->bhqd', p_own, v_own))
        return out.astype(q.dtype)

    starts = jnp.arange(S // Q_CHUNK) * Q_CHUNK
    outs = lax.map(chunk, starts)
    return outs.transpose(1, 2, 0, 3, 4).reshape(B, H, S, hd)


def multiscale_pool(p, w_mix, b_mix, layer_scale):
    B, S, C = p.shape
    pf = p.astype(jnp.float32).reshape(B, S, POOL_GROUPS, POOL_GROUP_DIM)
    cs0 = jnp.pad(jnp.cumsum(pf, axis=1), ((0, 0), (1, 0), (0, 0), (0, 0)))
    t = jnp.arange(S)
    pooled = []
    for g, w in enumerate(POOL_WINDOWS):
        csg = cs0[:, :, g]
        win = csg[:, 1:] - csg[:, jnp.maximum(t + 1 - w, 0)]
        cnt = jnp.minimum(t + 1, w).astype(jnp.float32)[None, :, None]
        pooled.append(win / cnt - pf[:, :, g])
    pooled = jnp.stack(pooled, axis=2)
    mixed = jnp.einsum('bsgc,gcd->bsgd', pooled, w_mix.astype(jnp.float32)) + b_mix.astype(jnp.float32)
    return (mixed.reshape(B, S, C) * layer_scale.astype(jnp.float32)).astype(p.dtype)


def peer_ffn(h, w_q, sub_keys, expert_u, expert_v):
    B, S, D = h.shape
    T = B * S
    ht = h.reshape(T, D)
    q = (ht @ w_q).reshape(T, PEER_HEADS, 2, PEER_HALF).astype(jnp.float32)
    s = jnp.einsum('thpd,pnd->thpn', q, sub_keys.astype(jnp.float32))
    v1, i1 = lax.top_k(s[:, :, 0], PEER_TOPK)
    v2, i2 = lax.top_k(s[:, :, 1], PEER_TOPK)
    cand = (v1[..., :, None] + v2[..., None, :]).reshape(T, PEER_HEADS, PEER_TOPK * PEER_TOPK)
    top_s, flat = lax.top_k(cand, PEER_TOPK)
    e1 = jnp.take_along_axis(i1, flat // PEER_TOPK, axis=-1)
    e2 = jnp.take_along_axis(i2, flat % PEER_TOPK, axis=-1)
    expert_idx = e1 * PEER_KEYS + e2
    gates = jax.nn.softmax(top_s, axis=-1)
    n_chunks = T // PEER_CHUNK

    def chunk(args):
        hc, idx, g = args
        a = jnp.einsum('cd,chkd->chk', hc, expert_u[idx]).astype(jnp.float32)
        act = (jax.nn.gelu(a) * g).astype(h.dtype)
        return jnp.einsum('chk,chkd->cd', act, expert_v[idx])

    out = lax.map(chunk, (ht.reshape(n_chunks, PEER_CHUNK, D),
                          expert_idx.reshape(n_chunks, PEER_CHUNK, PEER_HEADS, PEER_TOPK),
                          gates.reshape(n_chunks, PEER_CHUNK, PEER_HEADS, PEER_TOPK)))
    return out.reshape(B, S, D)


def setup_inputs(seed: int = 0) -> dict:
    key = jax.random.key(seed)
    ks = jax.random.split(key, 20)
    D = D_MODEL
    nrm = lambda k, shape, s: jax.random.normal(k, shape, dtype=jnp.float32) * s
    return {
        "x": nrm(ks[0], (BATCH, SEQ, D), 1.0),
        "c": nrm(ks[1], (BATCH, D), 1.0),
        "w_ada": nrm(ks[2], (DEPTH, D, 6 * D), 0.5 * D ** -0.5),
        "b_ada": nrm(ks[3], (DEPTH, 6 * D), 0.02),
        "g_norm1": 1.0 + nrm(ks[4], (DEPTH, D), 0.02),
        "w_in": nrm(ks[5], (DEPTH, D, IN_WIDTH), D ** -0.5),
        "w_attn_up": nrm(ks[6], (DEPTH, ATTN_WIDTH, D), ATTN_WIDTH ** -0.5),
        "w_pool_mix": nrm(ks[7], (DEPTH, POOL_GROUPS, POOL_GROUP_DIM, POOL_GROUP_DIM), POOL_GROUP_DIM ** -0.5),
        "b_pool_mix": nrm(ks[8], (DEPTH, POOL_GROUPS, POOL_GROUP_DIM), 0.02),
        "pool_scale": 1.0 + nrm(ks[9], (DEPTH, POOL_WIDTH), 0.05),
        "w_pool_up": nrm(ks[10], (DEPTH, POOL_WIDTH, D), POOL_WIDTH ** -0.5),
        "w_out": nrm(ks[11], (DEPTH, D, D), D ** -0.5),
        "g_norm2": 1.0 + nrm(ks[12], (DEPTH, D), 0.02),
        "w_peer_q": nrm(ks[13], (DEPTH, D, PEER_HEADS * PEER_QUERY_DIM), D ** -0.5),
        "peer_sub_keys": nrm(ks[14], (DEPTH, 2, PEER_KEYS, PEER_HALF), PEER_HALF ** -0.5),
        "peer_u": nrm(ks[15], (DEPTH, PEER_EXPERTS, D), D ** -0.5),
        "peer_v": nrm(ks[16], (DEPTH, PEER_EXPERTS, D), 0.5 * PEER_HEADS ** -0.5),
        "g_final": 1.0 + nrm(ks[17], (D,), 0.02),
    }


def reference(x, c, w_ada, b_ada, g_norm1, w_in, w_attn_up, w_pool_mix, b_pool_mix, pool_scale,
              w_pool_up, w_out, g_norm2, w_peer_q, peer_sub_keys, peer_u, peer_v, g_final):
    B, S, D = x.shape
    A = ATTN_WIDTH
    P = POOL_WIDTH
    split_at = [A, 2 * A, 3 * A, 3 * A + P, 3 * A + P + D]

    def heads(t):
        return t.reshape(B, S, ATTN_HEADS, ATTN_HEAD_DIM).transpose(0, 2, 1, 3)

    c_act = jax.nn.silu(c)
    for l in range(DEPTH):
        mod = c_act @ w_ada[l] + b_ada[l]
        sh1, sc1, gt1, sh2, sc2, gt2 = jnp.split(mod, 6, axis=-1)
        h = modulate(rms_norm(x, g_norm1[l]), sh1, sc1)
        proj = h @ w_in[l]
        q, k, v, p, ga, gb = jnp.split(proj, split_at, axis=-1)
        attn = moba_attention(heads(q), heads(k), heads(v)).transpose(0, 2, 1, 3).reshape(B, S, A)
        pool = multiscale_pool(p, w_pool_mix[l], b_pool_mix[l], pool_scale[l])
        merged = (jax.nn.sigmoid(ga) * (attn @ w_attn_up[l])
                  + jax.nn.sigmoid(gb) * (pool @ w_pool_up[l]))
        x = x + gt1[:, None, :] * (merged @ w_out[l])
        h2 = modulate(rms_norm(x, g_norm2[l]), sh2, sc2)
        x = x + gt2[:, None, :] * peer_ffn(h2, w_peer_q[l], peer_sub_keys[l], peer_u[l], peer_v[l])
    return rms_norm(x, g_final)
```

```python
import contextlib
import numpy as np
import ml_dtypes
import concourse.bass as bass
import concourse.mybir as mybir
from concourse.bass_utils import run_bass_kernel_spmd

F32 = mybir.dt.float32
BF16 = mybir.dt.bfloat16
U32 = mybir.dt.uint32
AF = mybir.ActivationFunctionType
ALU = mybir.AluOpType
AX = mybir.AxisListType

SEQ = 4096
D = 1024
NBLK = 16
BLK = 256
NEXP_CH = 128
EPS = 1e-6
NEG = -30000.0

ENGS = ["pe", "act", "dve", "pool", "sp"]
ENGMAP = {"pe": "tensor", "act": "scalar", "dve": "vector", "pool": "gpsimd", "sp": "sync"}


class Sched:
    LIM = 30000

    def __init__(self, nc):
        self.nc = nc
        self.ops = []
        self.last_write = {}
        self.readers = {}
        self.seen = set()
        self.bar = set()
        self.lastop = {}
        self.dma_ops = []

    def add(self, eng, fn, reads=(), writes=(), dma=False):
        deps = set()
        reads = list(reads)
        writes = list(writes)
        for b in list(reads):
            if b.startswith("bank") and b not in writes:
                writes.append(b)
        for b in list(reads) + list(writes):
            if b not in self.seen:
                self.seen.add(b)
                deps |= self.bar
        for b in reads:
            if b in self.last_write:
                deps.add(self.last_write[b])
        for b in writes:
            if b in self.last_write:
                deps.add(self.last_write[b])
            deps.update(self.readers.get(b, ()))
        idx = len(self.ops)
        self.ops.append(dict(eng=eng, fn=fn, deps=deps, dma=dma, sig=dma, sem=None, val=None))
        for b in reads:
            self.readers.setdefault(b, []).append(idx)
        for b in writes:
            self.last_write[b] = idx
            self.readers[b] = []
        self.lastop[(eng, dma)] = idx
        if dma:
            self.dma_ops.append(idx)
        return idx

    def barrier(self):
        self.bar = set(v for kk, v in self.lastop.items() if not kk[1]) | set(self.dma_ops[-self.NDMA:])
        self.seen = set()

    def pe(self, fn, reads=(), writes=()):
        return self.add("pe", fn, reads, writes)

    def act(self, fn, reads=(), writes=()):
        return self.add("act", fn, reads, writes)

    def dve(self, fn, reads=(), writes=()):
        return self.add("dve", fn, reads, writes)

    def pool(self, fn, reads=(), writes=()):
        return self.add("pool", fn, reads, writes)

    def dma(self, fn, reads=(), writes=(), q="sp"):
        return self.add(q, fn, reads, writes, dma=True)

    @staticmethod
    def _skip(src, op):
        return src["eng"] == "pe" and op["eng"] == "pe" and not src["dma"] and not op["dma"]

    NDMA = 32

    def emit(self, final_wait_eng="sp"):
        nc = self.nc
        ops = self.ops
        for op in ops:
            for d in op["deps"]:
                if not self._skip(ops[d], op):
                    ops[d]["sig"] = True
        cnt = {}
        epoch = {}
        semkeys = []
        ndma = 0
        for op in ops:
            if op["dma"]:
                slot = ndma % self.NDMA
                op["sem"] = ("dma", slot)
                op["val"] = 16 * (ndma // self.NDMA + 1)
                op["ev"] = (0, op["val"])
                ndma += 1
                continue
            if not op["sig"]:
                continue
            key = op["eng"]
            if key not in cnt:
                cnt[key] = 0
                epoch[key] = 0
                semkeys.append((key, 0))
            if cnt[key] + 1 > self.LIM:
                epoch[key] += 1
                cnt[key] = 0
                semkeys.append((key, epoch[key]))
            cnt[key] += 1
            op["sem"] = (key, epoch[key])
            op["val"] = cnt[key]
            op["ev"] = (epoch[key], cnt[key])
        for slot in range(min(ndma, self.NDMA)):
            semkeys.append(("dma", slot))
        with contextlib.ExitStack() as st:
            sems = {}
            for k in semkeys:
                sems[k] = st.enter_context(nc.semaphore("s_%s_%d" % (k[0], k[1])))
            block = st.enter_context(nc.Block())

            def make(engname):
                def body(e):
                    waited = {}

                    def wait(wk, ev):
                        if waited.get(wk, (-1, -1)) >= ev:
                            return
                        semk = wk if isinstance(wk, tuple) else (wk, ev[0])
                        e.wait_ge(sems[semk], ev[1])
                        waited[wk] = ev

                    for op in ops:
                        if op["eng"] != engname:
                            continue
                        need = {}
                        for d in op["deps"]:
                            src = ops[d]
                            if not src["sig"] or self._skip(src, op):
                                continue
                            wk = src["sem"] if src["dma"] else src["eng"]
                            if need.get(wk, (-1, -1)) < src["ev"]:
                                need[wk] = src["ev"]
                        if op["dma"] and op["val"] > 16:
                            wk = op["sem"]
                            ev = (0, op["val"] - 16)
                            if need.get(wk, (-1, -1)) < ev:
                                need[wk] = ev
                        for wk in sorted(need, key=str):
                            wait(wk, need[wk])
                        ins = op["fn"](e)
                        if op["dma"]:
                            ins.then_inc(sems[op["sem"]], 16)
                        elif op["sig"]:
                            ins.then_inc(sems[op["sem"]], 1)
                    if engname == final_wait_eng:
                        last = {}
                        for op in ops:
                            if op["dma"]:
                                last[op["sem"]] = op["ev"]
                        for wk in sorted(last, key=str):
                            wait(wk, last[wk])
                return body

            used = set(op["eng"] for op in ops) | {final_wait_eng}
            for engname in ENGS:
                if engname in used:
                    getattr(block, ENGMAP[engname])(make(engname))
        return len(semkeys)


class Rec:
    def __init__(self):
        self.l = []

    def pe(self, *a, **kw):
        self.l.append(("pe", a, kw))

    def act(self, *a, **kw):
        self.l.append(("act", a, kw))

    def dve(self, *a, **kw):
        self.l.append(("dve", a, kw))

    def pool(self, *a, **kw):
        self.l.append(("pool", a, kw))

    def dma(self, *a, **kw):
        self.l.append(("dma", a, kw))

    def replay(self, S, n=None):
        n = len(self.l) if n is None else min(n, len(self.l))
        for _ in range(n):
            m, a, kw = self.l.pop(0)
            getattr(S, m)(*a, **kw)


def host_consts():
    c = {}
    c["ident_f"] = np.eye(128, dtype=np.float32)
    slopes = 2.0 ** (-(np.arange(8) + 1.0))
    kc = np.zeros((18, SEQ), np.float32)
    kpos = np.arange(SEQ)
    for n in range(16):
        kc[n, kpos // BLK == n] = 1.0
    kc[16, :] = 1.0
    kc[17, :] = np.where((kpos % BLK) >= 128, 128.0, 0.0)
    c["kconst"] = kc.astype(ml_dtypes.bfloat16)
    qal = np.zeros((2, 8, BLK), np.float32)
    qi = np.arange(BLK)
    for h in range(8):
        qal[0, h, :] = -slopes[h] * qi
        qal[1, h, :] = slopes[h]
    c["qal"] = qal.astype(ml_dtypes.bfloat16)
    p = np.arange(128)
    bt = np.zeros((128, 8, 16), np.float32)
    for h in range(8):
        for df in range(16):
            bt[:, h, df] = slopes[h] * (p - 256.0 * df)
    c["biasT"] = bt
    cn = np.zeros((128, 2, BLK), np.float32)
    for kh in range(2):
        ki = 128 * kh + p
        cn[:, kh, :] = np.where(ki[:, None] > qi[None, :], NEG, 0.0)
    c["causalneg"] = cn
    mb = np.zeros((128, 16, 16), np.float32)
    for b in range(16):
        mb[:, b, b:] = -1e30
    c["maskb"] = mb
    ic = np.zeros((128, 4, 16), np.float32)
    for g, w in enumerate((2, 4, 8, 16)):
        ic[:, g, :] = 1.0 / np.minimum(np.arange(16) + 1, w)
    c["invcnt16"] = ic
    c["iota128"] = np.tile(np.arange(128, dtype=np.float32)[None, :], (128, 1))
    c["iota16"] = np.tile(np.arange(16, dtype=np.float32)[None, :], (128, 1))
    return c


class K:
    pass


def build(dbg=None, phases="AU1x2", opts=None):
    nc = bass.Bass("TRN2", target_bir_lowering=False)
    k = K()
    k.nc = nc
    k.S = Sched(nc)
    k.dbg = dbg or ()
    k.opts = opts or {}
    S = k.S

    def din(name, shape, dt=F32):
        return nc.dram_tensor(name, list(shape), dt, kind="ExternalInput").ap()

    d = {}
    d["x"] = din("x", [SEQ, D])
    d["c"] = din("c", [128, 8])
    d["w_ada"] = din("w_ada", [D, 6 * D])
    d["b_ada"] = din("b_ada", [1, 6 * D])
    d["g1"] = din("g1", [1, D])
    d["g2"] = din("g2", [1, D])
    d["gf"] = din("gf", [1, D])
    d["w_in"] = din("w_in", [D, 4096])
    d["w_au"] = din("w_au", [512, D])
    d["w_mix"] = din("w_mix", [4, 128, 128])
    d["b_mix"] = din("b_mix", [128, 4])
    d["pscale"] = din("pscale", [128, 4])
    d["w_pu"] = din("w_pu", [512, D])
    d["w_out"] = din("w_out", [D, D])
    d["w_pq"] = din("w_pq", [D, 2048])
    d["keys"] = din("keys", [2, 128, 128])
    d["peer_u"] = din("peer_u", [16384, D])
    d["peer_v"] = din("peer_v", [16384, D])
    hc = host_consts()
    for nm, arr in hc.items():
        d[nm] = din(nm, arr.shape, BF16 if arr.dtype == ml_dtypes.bfloat16 else F32)
    d["out"] = nc.dram_tensor("out", [SEQ, D], F32, kind="ExternalOutput").ap()
    d["x1s"] = nc.dram_tensor("x1s", [SEQ, D], F32, kind="Internal").ap()
    d["uts"] = nc.dram_tensor("uts", [128, 128, 1024], BF16, kind="Internal").ap()
    d["vs"] = nc.dram_tensor("vs", [128, 128, 1024], BF16, kind="Internal").ap()
    d["bcs"] = nc.dram_tensor("bcs", [3, 128, D], F32, kind="Internal").ap()
    k.d = d
    k.dbg_out = {}

    def dbg_tensor(name, shape, dt=F32):
        k.dbg_out[name] = nc.dram_tensor("dbg_" + name, list(shape), dt, kind="ExternalOutput").ap()
        return k.dbg_out[name]

    k.dbg_tensor = dbg_tensor

    with contextlib.ExitStack() as st0:
        def T(name, shape, dt=F32, st=st0):
            return st.enter_context(nc.sbuf_tensor("t_" + name, list(shape), dt))

        k.T = T
        k.bank = [st0.enter_context(nc.psum_tensor("bank%d" % i, [128, 512], F32)) for i in range(8)]
        k.ident = T("ident", [128, 128])
        k.modcols = T("modcols", [128, 32])
        S.dma(lambda e: e.dma_start(out=k.ident[:], in_=d["ident_f"]), writes=["ident"])
        if "A" in phases:
            phase_A(k)
        S.barrier()
        if "U" in phases and not k.opts.get("u_in_1b", True):
            phase_U(k)
        S.barrier()
        if "1" in phases:
            with contextlib.ExitStack() as st1:
                k.attnT = T("attnT", [128, 4, SEQ], BF16, st1)
                if "1a" in phases or "1x" in phases:
                    phase_1a(k)
                S.barrier()
                if "1b" in phases or "1x" in phases:
                    phase_1b(k)
        S.barrier()
        if "2" in phases:
            phase_2(k)
        nsem = S.emit()
    k.nsem = nsem
    return nc, k


def phase_A(k):
    nc, S, d = k.nc, k.S, k.d
    with contextlib.ExitStack() as st:
        T = lambda n, s, dt=F32: k.T(n, s, dt, st)
        ccol = T("ccol", [128, 8])
        cact = T("cact", [128, 8])
        wt = [T("wadaA", [128, 8, 512]), T("wadaB", [128, 8, 512])]
        modrow = T("modrow", [1, 6 * D])
        badar = T("badar", [1, 6 * D])
        g1r = T("g1r", [1, D])
        g2r = T("g2r", [1, D])
        gfr = T("gfr", [1, D])
        rows = T("rows", [1, 4, D])
        onesr = T("onesr", [1, 128])
        S.dma(lambda e: e.dma_start(out=ccol[:], in_=d["c"]), writes=["ccol"])
        S.act(lambda e: e.activation(out=cact[:], in_=ccol[:], func=AF.Silu), reads=["ccol"], writes=["cact"])
        S.dma(lambda e: e.dma_start(out=badar[:], in_=d["b_ada"]), writes=["badar"])
        S.dma(lambda e: e.dma_start(out=g1r[:], in_=d["g1"]), writes=["g1r"])
        S.dma(lambda e: e.dma_start(out=g2r[:], in_=d["g2"]), writes=["g2r"])
        S.dma(lambda e: e.dma_start(out=gfr[:], in_=d["gf"]), writes=["gfr"])
        S.dve(lambda e: e.memset(onesr[:], 1.0), writes=["onesr"])
        wv = d["w_ada"].rearrange("(kc p) n -> p kc n", p=128)
        for nch in range(12):
            w = wt[nch % 2]
            wn = "wada%d" % (nch % 2)
            pn = "bank%d" % (nch % 2)
            ps = k.bank[nch % 2]
            S.dma(lambda e, w=w, nch=nch: e.dma_start(out=w[:], in_=wv[:, :, nch * 512:(nch + 1) * 512]), writes=[wn])
            for kc in range(8):
                S.pe(lambda e, ps=ps, w=w, kc=kc: e.matmul(ps[0:1, :], lhsT=cact[:, kc:kc + 1], rhs=w[:, kc, :], start=(kc == 0), stop=(kc == 7)),
                     reads=["cact", wn], writes=[pn])
            S.dve(lambda e, ps=ps, nch=nch: e.tensor_tensor(out=modrow[0:1, nch * 512:(nch + 1) * 512], in0=ps[0:1, :], in1=badar[0:1, nch * 512:(nch + 1) * 512], op=ALU.add),
                  reads=[pn, "badar"], writes=["modrow"])
        S.dve(lambda e: e.scalar_tensor_tensor(out=rows[0:1, 0, :], in0=modrow[0:1, D:2 * D], scalar=1.0, in1=g1r[0:1, :], op0=ALU.add, op1=ALU.mult),
              reads=["modrow", "g1r"], writes=["rows"])
        S.dve(lambda e: e.tensor_copy(out=rows[0:1, 1, :], in_=modrow[0:1, 0:D]), reads=["modrow"], writes=["rows"])
        S.dve(lambda e: e.scalar_tensor_tensor(out=rows[0:1, 2, :], in0=modrow[0:1, 4 * D:5 * D], scalar=1.0, in1=g2r[0:1, :], op0=ALU.add, op1=ALU.mult),
              reads=["modrow", "g2r"], writes=["rows"])
        S.dve(lambda e: e.tensor_copy(out=rows[0:1, 3, :], in_=modrow[0:1, 3 * D:4 * D]), reads=["modrow"], writes=["rows"])
        colps = k.bank[2]
        for v in range(4):
            for kc in range(8):
                S.pe(lambda e, v=v, kc=kc: e.matmul(colps[:, v * 8 + kc:v * 8 + kc + 1], lhsT=rows[0:1, v, kc * 128:(kc + 1) * 128], rhs=onesr[0:1, 0:1], start=True, stop=True),
                     reads=["rows", "onesr"], writes=["bank2"])
        S.dve(lambda e: e.tensor_copy(out=k.modcols[:], in_=colps[:, 0:32]), reads=["bank2"], writes=["modcols"])
        bct = [T("bct0", [128, D]), T("bct1", [128, D]), T("bct2", [128, D])]
        srcs = [(bct[0], "bct0", modrow, 2 * D, "modrow"), (bct[1], "bct1", modrow, 5 * D, "modrow"), (bct[2], "bct2", gfr, 0, "gfr")]
        i = 0
        for dst, dn, src, off, sn in srcs:
            for half in range(2):
                bi = 3 + (i % 2)
                i += 1
                ps = k.bank[bi]
                S.pe(lambda e, ps=ps, src=src, off=off, half=half: e.matmul(ps[:, :], lhsT=onesr[0:1, :], rhs=src[0:1, off + half * 512:off + (half + 1) * 512], start=True, stop=True),
                     reads=["onesr", sn], writes=["bank%d" % bi])
                S.act(lambda e, ps=ps, dst=dst, half=half: e.copy(out=dst[:, half * 512:(half + 1) * 512], in_=ps[:, :]), reads=["bank%d" % bi], writes=[dn])
        for j in range(3):
            S.dma(lambda e, j=j: e.dma_start(out=d["bcs"][j], in_=bct[j][:]), reads=["bct%d" % j], writes=["bcs%d" % j])
        if "A" in k.dbg:
            o1 = k.dbg_tensor("modcols", [128, 32])
            o2 = k.dbg_tensor("gt1b", [128, D])
            S.dma(lambda e: e.dma_start(out=o1, in_=k.modcols[:]), reads=["modcols"])
            S.dma(lambda e: e.dma_start(out=o2, in_=bct[0][:]), reads=["bct0"])


def phase_U(k):
    nc, S, d = k.nc, k.S, k.d
    with contextlib.ExitStack() as st:
        T = lambda n, s, dt=F32: k.T(n, s, dt, st)
        ut = [T("ut0", [128, D]), T("ut1", [128, D])]
        vt = [T("vt0", [128, D]), T("vt1", [128, D])]
        utc = [T("utc0", [128, 8, 128], BF16), T("utc1", [128, 8, 128], BF16)]
        vbf = [T("vbf0", [128, D], BF16), T("vbf1", [128, D], BF16)]
        for i in range(128):
            j = i % 2
            S.dma(lambda e, i=i, j=j: e.dma_start(out=ut[j][:], in_=d["peer_u"][i * 128:(i + 1) * 128, :]), writes=["ut%d" % j])
            S.dma(lambda e, i=i, j=j: e.dma_start(out=vt[j][:], in_=d["peer_v"][i * 128:(i + 1) * 128, :]), writes=["vt%d" % j])
            for half in range(2):
                bi = 2 * j + half
                ps = k.bank[bi]
                for q in range(4):
                    kc = half * 4 + q
                    S.pe(lambda e, ps=ps, q=q, kc=kc, j=j: e.transpose(ps[:, q * 128:(q + 1) * 128], ut[j][:, kc * 128:(kc + 1) * 128], k.ident[:]),
                         reads=["ut%d" % j, "ident"], writes=["bank%d" % bi])
                fn = lambda e, ps=ps, half=half, j=j: e.tensor_copy(out=utc[j][:, half * 4:(half + 1) * 4, :], in_=ps[:, :].rearrange("p (a b) -> p a b", b=128))
                fa = lambda e, ps=ps, half=half, j=j: e.copy(out=utc[j][:, half * 4:(half + 1) * 4, :], in_=ps[:, :].rearrange("p (a b) -> p a b", b=128))
                if half == 0:
                    S.dve(fn, reads=["bank%d" % bi], writes=["utc%d" % j])
                else:
                    S.act(fa, reads=["bank%d" % bi], writes=["utc%d" % j])
            S.dma(lambda e, i=i, j=j: e.dma_start(out=d["uts"][:, i, :], in_=utc[j][:].rearrange("p a b -> p (a b)")), reads=["utc%d" % j], writes=["uts%d" % (i // 2)])
            S.pool(lambda e, j=j: e.tensor_copy(out=vbf[j][:], in_=vt[j][:]), reads=["vt%d" % j], writes=["vbf%d" % j])
            S.dma(lambda e, i=i, j=j: e.dma_start(out=d["vs"][:, i, :], in_=vbf[j][:]), reads=["vbf%d" % j], writes=["vs%d" % (i // 2)])


def u_load(k, S, i, ut, vt, names):
    d = k.d
    nut, nvt = names[0], names[1]
    S.dma(lambda e: e.dma_start(out=ut[:], in_=d["peer_u"][i * 128:(i + 1) * 128, :]), writes=[nut])
    S.dma(lambda e: e.dma_start(out=vt[:], in_=d["peer_v"][i * 128:(i + 1) * 128, :]), writes=[nvt])


def u_proc(k, S, i, ut, vt, utc, vbf, banks, names):
    d = k.d
    nut, nvt, nutc, nvbf = names
    for half in range(2):
        bi = banks[half]
        ps = k.bank[bi]
        for q in range(4):
            kc = half * 4 + q
            S.pe(lambda e, ps=ps, q=q, kc=kc: e.transpose(ps[:, q * 128:(q + 1) * 128], ut[:, kc * 128:(kc + 1) * 128], k.ident[:]), reads=[nut, "ident"], writes=["bank%d" % bi])
        if half == 0:
            S.dve(lambda e, ps=ps: e.tensor_copy(out=utc[:, 0:4, :], in_=ps[:, :].rearrange("p (a b) -> p a b", b=128)), reads=["bank%d" % bi], writes=[nutc])
        else:
            S.act(lambda e, ps=ps: e.copy(out=utc[:, 4:8, :], in_=ps[:, :].rearrange("p (a b) -> p a b", b=128)), reads=["bank%d" % bi], writes=[nutc])
    S.dma(lambda e: e.dma_start(out=d["uts"][:, i, :], in_=utc[:].rearrange("p a b -> p (a b)")), reads=[nutc], writes=["uts%d" % (i // 2)], q="act")
    S.act(lambda e: e.copy(out=vbf[:], in_=vt[:]), reads=[nvt], writes=[nvbf])
    S.dma(lambda e: e.dma_start(out=d["vs"][:, i, :], in_=vbf[:]), reads=[nvbf], writes=["vs%d" % (i // 2)], q="act")


def norm_block(k, T2, src, rows0, acol, bcol, hT, hname, tag, src_deps=None, S=None, banks=(0, 1)):
    S = S or k.S
    for tt in range(2):
        xt = T2["xt"][tt]
        xn_ = T2["xtn"][tt] if "xtn" in T2 else "xt%s%d" % (tag, tt)
        S.dma(lambda e, xt=xt, tt=tt: e.dma_start(out=xt[:], in_=src[rows0 + tt * 128:rows0 + (tt + 1) * 128, :]), reads=([src_deps[tt]] if src_deps else []), writes=[xn_])
        junk = T2["junk"]
        ss = T2["ss"]
        xn = T2["xn"]
        S.act(lambda e, xt=xt, tt=tt: e.activation(out=junk[:], in_=xt[:], func=AF.Square, accum_out=ss[:, tt:tt + 1]), reads=[xn_], writes=[T2.get("junkn", "junk" + tag), "ss%s%d" % (tag, tt)])
        S.dve(lambda e, tt=tt: e.tensor_scalar(out=ss[:, tt:tt + 1], in0=ss[:, tt:tt + 1], scalar1=1.0 / D, scalar2=EPS, op0=ALU.mult, op1=ALU.add),
              reads=["ss%s%d" % (tag, tt)], writes=["ss%s%d" % (tag, tt)])
        S.act(lambda e, tt=tt: e.activation(out=ss[:, tt:tt + 1], in_=ss[:, tt:tt + 1], func=AF.Sqrt),
              reads=["ss%s%d" % (tag, tt)], writes=["ss%s%d" % (tag, tt)])
        S.dve(lambda e, tt=tt: e.reciprocal(out=ss[:, tt:tt + 1], in_=ss[:, tt:tt + 1]),
              reads=["ss%s%d" % (tag, tt)], writes=["ss%s%d" % (tag, tt)])
        S.act(lambda e, xt=xt, tt=tt: e.activation(out=xn[:], in_=xt[:], func=AF.Copy, scale=ss[:, tt:tt + 1]), reads=[xn_, "ss%s%d" % (tag, tt)], writes=["xn" + tag])
        for half in range(2):
            ps = k.bank[banks[half]]
            for q in range(4):
                kc = half * 4 + q
                S.pe(lambda e, ps=ps, q=q, kc=kc: e.transpose(ps[:, q * 128:(q + 1) * 128], xn[:, kc * 128:(kc + 1) * 128], k.ident[:]),
                     reads=["xn" + tag, "ident"], writes=["bank%d" % banks[half]])
            tmp = T2["tmp"]
            S.dve(lambda e, ps=ps, half=half: e.tensor_tensor(out=tmp[:], in0=ps[:, :].rearrange("p (a b) -> p a b", b=128),
                                                              in1=acol[:, half * 4:(half + 1) * 4].unsqueeze(2).to_broadcast([128, 4, 128]), op=ALU.mult),
                  reads=["bank%d" % banks[half], "modcols"], writes=["ntmp" + tag])
            S.dve(lambda e, half=half, tt=tt: e.tensor_tensor(out=hT[:, half * 4:(half + 1) * 4, tt * 128:(tt + 1) * 128], in0=tmp[:],
                                                               in1=bcol[:, half * 4:(half + 1) * 4].unsqueeze(2).to_broadcast([128, 4, 128]), op=ALU.add),
                  reads=["ntmp" + tag, "modcols"], writes=[hname])


def load_cast(k, dst, dname, src_ap, stage, sname, eng="dve"):
    S = k.S
    S.dma(lambda e: e.dma_start(out=stage, in_=src_ap), writes=[sname])
    if eng == "dve":
        S.dve(lambda e: e.tensor_copy(out=dst, in_=stage), reads=[sname], writes=[dname])
    elif eng == "pool":
        S.pool(lambda e: e.tensor_copy(out=dst, in_=stage), reads=[sname], writes=[dname])
    else:
        S.act(lambda e: e.copy(out=dst, in_=stage), reads=[sname], writes=[dname])


def phase_1a(k):
    nc, S, d = k.nc, k.S, k.d
    with contextlib.ExitStack() as st:
        T = lambda n, s, dt=F32: k.T(n, s, dt, st)
        wqkv = T("wqkv", [128, 8, 1536], BF16)
        kaug = T("kaug", [128, 8, SEQ], BF16)
        vaug = T("vaug", [128, 32, 8, 65], BF16)
        qaug = [T("qaug0", [128, 8, BLK], BF16), T("qaug1", [128, 8, BLK], BF16)]
        qf = T("qf", [64, 8, BLK])
        kms = T("kms", [64, 8, 16])
        kmf = T("kmf", [64, 8, 16])
        xtA = T("xtA0", [128, D])
        xnA = T("xnA", [128, D])
        T2 = dict(xt=[xtA, xtA], junk=xnA, ss=T("ssA", [128, 2]), xn=xnA, tmp=T("ntmpA", [128, 4, 128]), xtn=["xtA0", "xtA0"], junkn="xnA")
        stage = [xtA[:, 0:768], xnA[:, 0:768]]
        hT = T("hTA", [128, 8, BLK], BF16)
        biasT = T("biasT", [128, 8, 16])
        causalneg = T("causalneg", [128, 2, BLK])
        maskb = T("maskb", [128, 16, 16])
        gm = T("gm", [128, 8, 16])
        mx8 = T("mx8", [128, 8, 8])
        Wall = T("Wall", [128, 8, 128])
        ssb = T("ssb", [128, 2, BLK])
        PT = [T("PT%d" % i, [128, 2, BLK], BF16) for i in range(3)]
        rinv = T("rinv", [128, 2])
        attn_tok = T("attn_tok", [128, 2, 512])

        for kc in range(8):
            for hf in range(2):
                load_cast(k, wqkv[:, kc, hf * 768:(hf + 1) * 768], "wqkv", d["w_in"][kc * 128:(kc + 1) * 128, hf * 768:(hf + 1) * 768], stage[hf], ("xtA0" if hf == 0 else "xnA"), eng=("dve" if hf == 0 else "pool"))
        S.dma(lambda e: e.dma_start(out=biasT[:], in_=d["biasT"]), writes=["biasT"])
        S.dma(lambda e: e.dma_start(out=causalneg[:], in_=d["causalneg"]), writes=["causalneg"])
        S.dma(lambda e: e.dma_start(out=maskb[:], in_=d["maskb"]), writes=["maskb"])
        for h in range(8):
            S.dma(lambda e, h=h: e.dma_start(out=kaug[64:82, h, :], in_=d["kconst"]), writes=["kaug_c"])
        for j in range(2):
            S.dma(lambda e, j=j: e.dma_start(out=qaug[j][80:82, :, :], in_=d["qal"]), writes=["qaug%d_c" % j])
        S.dve(lambda e: e.memset(vaug[:, :, :, 64:65], 1.0), writes=["vaug_c"])
        S.dve(lambda e: e.memset(Wall[:], 0.0), writes=["Wall"])
        S.dve(lambda e: e.memset(kms[:], 0.0), writes=["kms"])
        S.dve(lambda e: e.memset(kmf[:], 0.0), writes=["kmf"])
        S.pool(lambda e: e.memset(qaug[0][64:80, :, :], 0.0), writes=["qaug0_s"])

        acol = k.modcols[:, 0:8]
        bcol = k.modcols[:, 8:16]
        hTs = [hT, T("hTA1", [128, 8, BLK], BF16)]
        nb_run = k.opts.get("nb1a", NBLK)

        def block_prelude(S, b):
            qa = qaug[b % 2]
            qan = "qaug%d" % (b % 2)
            hTb = hTs[b % 2]
            hn = "hTA%d" % (b % 2)
            norm_block(k, T2, d["x"], b * BLK, acol, bcol, hTb, hn, "A", S=S)
            for h in range(8):
                qps = k.bank[2][0:64, 0:256]
                kps = k.bank[3][0:64, 0:256]
                for kc in range(8):
                    S.pe(lambda e, h=h, kc=kc, qps=qps: e.matmul(qps, lhsT=wqkv[:, kc, h * 64:(h + 1) * 64], rhs=hTb[:, kc, :], start=(kc == 0), stop=(kc == 7)),
                         reads=["wqkv", hn], writes=["bank2"])
                S.dve(lambda e, h=h, qps=qps: e.tensor_scalar(out=qf[:, h, :], in0=qps, scalar1=0.125, scalar2=None, op0=ALU.mult), reads=["bank2"], writes=["qf"])
                S.pool(lambda e, h=h, qa=qa: e.tensor_copy(out=qa[0:64, h, :], in_=qf[:, h, :]), reads=["qf"], writes=[qan + "_q"])
                for kc in range(8):
                    S.pe(lambda e, h=h, kc=kc, kps=kps: e.matmul(kps, lhsT=wqkv[:, kc, 512 + h * 64:512 + (h + 1) * 64], rhs=hTb[:, kc, :], start=(kc == 0), stop=(kc == 7)),
                         reads=["wqkv", hn], writes=["bank3"])
                S.act(lambda e, h=h, b=b, kps=kps: e.copy(out=kaug[0:64, h, b * BLK:(b + 1) * BLK], in_=kps), reads=["bank3"], writes=["kaug_%d" % b])
                S.dve(lambda e, h=h, b=b, kps=kps: e.tensor_reduce(out=kms[:, h, b:b + 1], in_=kps, axis=AX.X, op=ALU.add), reads=["bank3"], writes=["kms"])
            S.dve(lambda e, b=b: e.tensor_scalar(out=kmf[:, :, b:b + 1], in0=kms[:, :, b:b + 1], scalar1=1.0 / BLK, scalar2=None, op0=ALU.mult), reads=["kms"], writes=["kmf"])
            for tt in range(2):
                vps = k.bank[2 + tt]
                vpn = "bank%d" % (2 + tt)
                for kc in range(8):
                    S.pe(lambda e, tt=tt, kc=kc, vps=vps: e.matmul(vps[:, :], lhsT=hTb[:, kc, tt * 128:(tt + 1) * 128], rhs=wqkv[:, kc, 1024:1536], start=(kc == 0), stop=(kc == 7)),
                         reads=["wqkv", hn], writes=[vpn])
                S.act(lambda e, tt=tt, b=b, vps=vps: e.copy(out=vaug[:, 2 * b + tt, :, 0:64], in_=vps[:, :].rearrange("p (h c) -> p h c", c=64)), reads=[vpn], writes=["vaug_%d" % b])
            if b == 0:
                return
            for tt in range(2):
                gps = k.bank[0][:, 0:128].rearrange("p (h n) -> p h n", n=16)
                for h in range(8):
                    S.pe(lambda e, h=h, tt=tt, gps=gps: e.matmul(gps[:, h, :], lhsT=qf[:, h, tt * 128:(tt + 1) * 128], rhs=kmf[:, h, :], start=True, stop=True),
                         reads=["qf", "kmf"], writes=["bank0"])
                S.dve(lambda e, b=b, gps=gps: e.tensor_tensor(out=gm[:], in0=gps, in1=maskb[:, b, :].unsqueeze(1).to_broadcast([128, 8, 16]), op=ALU.add),
                      reads=["bank0", "maskb"], writes=["gm"])
                for h in range(8):
                    S.dve(lambda e, h=h: e.max(out=mx8[:, h, :], in_=gm[:, h, :]), reads=["gm"], writes=["mx8"])
                S.dve(lambda e: e.tensor_tensor(out=Wall[:, :, 64:80], in0=gm[:], in1=mx8[:, :, 2:3].to_broadcast([128, 8, 16]), op=ALU.is_ge),
                      reads=["gm", "mx8"], writes=["Wall"])
                S.dve(lambda e: e.tensor_scalar(out=Wall[:, :, 64:80], in0=Wall[:, :, 64:80], scalar1=-NEG, scalar2=NEG, op0=ALU.mult, op1=ALU.add),
                      reads=["Wall"], writes=["Wall"])
                S.dve(lambda e, b=b: e.memset(Wall[:, :, 64 + b:65 + b], 0.0), reads=["Wall"], writes=["Wall"])
                for hg in range(2):
                    wtp = k.bank[2 + hg]
                    wtn = "bank%d" % (2 + hg)
                    for j in range(4):
                        S.pe(lambda e, hg=hg, j=j, wtp=wtp: e.transpose(wtp[:, j * 128:(j + 1) * 128], Wall[:, hg * 4 + j, :], k.ident[:]), reads=["Wall", "ident"], writes=[wtn])
                    S.act(lambda e, hg=hg, tt=tt, qa=qa, wtp=wtp: e.copy(out=qa[64:80, hg * 4:(hg + 1) * 4, tt * 128:(tt + 1) * 128],
                                                                in_=wtp[64:80, :].rearrange("p (a b) -> p a b", b=128)), reads=[wtn], writes=[qan + "_s"])

        def block_attention(b, rec):
            qa = qaug[b % 2]
            qan = "qaug%d" % (b % 2)
            units = [(h, n) for h in range(8) for n in range(b + 1)]
            per = (len(rec.l) + len(units) - 1) // len(units)

            def unit_S(ui):
                h, n = units[ui]
                sb_i = 6 + (ui % 2)
                stp = k.bank[sb_i]
                for kh in range(2):
                    S.pe(lambda e, h=h, n=n, kh=kh, stp=stp: e.matmul(stp[:, kh * 256:(kh + 1) * 256], lhsT=kaug[0:82, h, (2 * n + kh) * 128:(2 * n + kh + 1) * 128],
                                                                     rhs=qa[0:82, h, :], start=True, stop=True),
                         reads=["kaug_c", "kaug_%d" % n, qan + "_q", qan + "_s", qan + "_c"], writes=["bank%d" % sb_i])

            def unit_EV(ui):
                h, n = units[ui]
                sb_i = 6 + (ui % 2)
                stp = k.bank[sb_i]
                stn = "bank%d" % sb_i
                pt = PT[ui % 3]
                ptn = "PT%d" % (ui % 3)
                acc = (k.bank[4][:, 0:130] if h % 2 == 0 else k.bank[5][:, 256:386]).rearrange("p (t c) -> p t c", c=65)
                accn = "bank%d" % (4 + h % 2)
                if n == b:
                    S.dve(lambda e, stp=stp: e.tensor_tensor(out=ssb[:].rearrange("p a b -> p (a b)"), in0=stp[:, :], in1=causalneg[:].rearrange("p a b -> p (a b)"), op=ALU.add),
                          reads=[stn, "causalneg"], writes=["ssb"])
                    S.act(lambda e, pt=pt, h=h: e.activation(out=pt[:].rearrange("p a b -> p (a b)"), in_=ssb[:].rearrange("p a b -> p (a b)"), func=AF.Exp, bias=biasT[:, h, 0:1]),
                          reads=["ssb", "biasT"], writes=[ptn])
                else:
                    S.act(lambda e, pt=pt, h=h, stp=stp, df=b - n: e.activation(out=pt[:].rearrange("p a b -> p (a b)"), in_=stp[:, :], func=AF.Exp, bias=biasT[:, h, df:df + 1]),
                          reads=[stn, "biasT"], writes=[ptn])
                for kh in range(2):
                    for qt in range(2):
                        S.pe(lambda e, pt=pt, kh=kh, qt=qt, n=n, h=h, acc=acc: e.matmul(acc[:, qt, :], lhsT=pt[:, kh, qt * 128:(qt + 1) * 128], rhs=vaug[:, 2 * n + kh, h, :],
                                                                                      start=(n == 0 and kh == 0 and qt == 0), stop=(n == b and kh == 1), skip_group_check=True),
                             reads=[ptn, "vaug_%d" % n, "vaug_c"], writes=[accn])
                if n == b:
                    S.dve(lambda e, acc=acc: e.reciprocal(out=rinv[:], in_=acc[:, :, 64]), reads=[accn], writes=["rinv"])
                    S.dve(lambda e, acc=acc, h=h: e.tensor_tensor(out=attn_tok[:, :, h * 64:(h + 1) * 64], in0=acc[:, :, 0:64], in1=rinv[:].unsqueeze(2).to_broadcast([128, 2, 64]), op=ALU.mult),
                          reads=[accn, "rinv"], writes=["attn_tok"])

            unit_S(0)
            for ui in range(len(units)):
                if ui + 1 < len(units):
                    unit_S(ui + 1)
                unit_EV(ui)
                rec.replay(S, per)
            rec.replay(S)
            for qt in range(2):
                tp = k.bank[2 + qt]
                tpn = "bank%d" % (2 + qt)
                for ac in range(4):
                    S.pe(lambda e, qt=qt, ac=ac, tp=tp: e.transpose(tp[:, ac * 128:(ac + 1) * 128], attn_tok[:, qt, ac * 128:(ac + 1) * 128], k.ident[:]), reads=["attn_tok", "ident"], writes=[tpn])
                S.act(lambda e, qt=qt, b=b, tp=tp: e.copy(out=k.attnT[:, :, b * BLK + qt * 128:b * BLK + (qt + 1) * 128], in_=tp[:, :].rearrange("p (a b) -> p a b", b=128)),
                      reads=[tpn], writes=["attnT_%d" % b])

        block_prelude(S, 0)
        for b in range(nb_run):
            rec = Rec()
            if b + 1 < nb_run:
                block_prelude(rec, b + 1)
            block_attention(b, rec)

        if "1a" in k.dbg:
            o = k.dbg_tensor("attnT", [128, 4, SEQ], BF16)
            nb_ = k.opts.get("nb1a", NBLK)
            S.dma(lambda e: e.dma_start(out=o, in_=k.attnT[:]), reads=["attnT_%d" % b for b in range(nb_)])
            o2 = k.dbg_tensor("kaug", [128, 8, SEQ], BF16)
            S.dma(lambda e: e.dma_start(out=o2, in_=kaug[:]), reads=["kaug_c"] + ["kaug_%d" % b for b in range(nb_)])
            o3 = k.dbg_tensor("qaug1", [128, 8, BLK], BF16)
            qi_ = (nb_ - 1) % 2
            S.dma(lambda e: e.dma_start(out=o3, in_=qaug[qi_][:]), reads=["qaug%d_q" % qi_, "qaug%d_s" % qi_, "qaug%d_c" % qi_])
            o5 = k.dbg_tensor("attn_tok", [128, 2, 512])
            S.dma(lambda e: e.dma_start(out=o5, in_=attn_tok[:]), reads=["attn_tok"])
            o6 = k.dbg_tensor("PT0", [128, 2, BLK], BF16)
            S.dma(lambda e: e.dma_start(out=o6, in_=PT[0][:]), reads=["PT0"])
            o7 = k.dbg_tensor("ssb", [128, 2, BLK])
            S.dma(lambda e: e.dma_start(out=o7, in_=ssb[:]), reads=["ssb"])
            o8 = k.dbg_tensor("rinv", [128, 2])
            S.dma(lambda e: e.dma_start(out=o8, in_=rinv[:]), reads=["rinv"])
            o4 = k.dbg_tensor("vaug", [128, 32, 8, 65], BF16)
            S.dma(lambda e: e.dma_start(out=o4, in_=vaug[:]), reads=["vaug_c"] + ["vaug_%d" % b for b in range(nb_)])


def phase_1b(k):
    nc, S, d = k.nc, k.S, k.d
    with contextlib.ExitStack() as st:
        T = lambda n, s, dt=F32: k.T(n, s, dt, st)
        wpg = T("wpg", [128, 8, 2560], BF16)
        stage = [T("wstb0", [128, 1280]), T("wstb1", [128, 1280])]
        wau = T("wau", [128, 4, D], BF16)
        wpu = T("wpu", [128, 4, D], BF16)
        wout = T("wout", [128, 8, D], BF16)
        wmix = T("wmix", [128, 4, 128], BF16)
        bmix = T("bmixc", [128, 4])
        pscale = T("pscalec", [128, 4])
        invc = T("invc", [128, 4, 16])
        T2 = dict(xt=[T("xtB0", [128, D]), T("xtB1", [128, D])], junk=T("junkB", [128, D], BF16), ss=T("ssB", [128, 2]), xn=T("xnB", [128, D]), tmp=T("ntmpB", [128, 4, 128]))
        hT = T("hTB", [128, 8, BLK], BF16)
        pbuf = [T("pbuf0", [128, 4, 272]), T("pbuf1", [128, 4, 272])]
        s2 = T("s2", [128, 4, 272])
        s4 = T("s4", [128, 4, 272])
        s8 = T("s8", [128, 4, 272])
        s16 = T("s16", [128, 4, 272])
        t16 = T("t16", [128, 16])
        pin = T("pin", [128, 4, BLK], BF16)
        poolT = T("poolT", [128, 4, BLK], BF16)
        sga = [T("sga0", [128, BLK]), T("sga1", [128, BLK])]
        sgb = [T("sgb0", [128, BLK]), T("sgb1", [128, BLK])]
        m1 = T("m1", [128, BLK])
        m2 = T("m2", [128, BLK])
        mergedT = T("mergedT", [128, 8, BLK], BF16)
        x1t = [T("x1t0", [128, D]), T("x1t1", [128, D])]
        u_ut = [T("u_ut0", [128, D]), T("u_ut1", [128, D])]
        u_vt = [T("u_vt0", [128, D]), T("u_vt1", [128, D])]
        u_utc = T("u_utc", [128, 8, 128], BF16)
        u_vbf = T("u_vbf", [128, D], BF16)
        u_next = [0]
        gt1b = T("gt1b", [128, D])
        S.dma(lambda e: e.dma_start(out=gt1b[:], in_=d["bcs"][0]), reads=["bcs0"], writes=["gt1b"])

        for kc in range(8):
            for hf in range(2):
                load_cast(k, wpg[:, kc, hf * 1280:(hf + 1) * 1280], "wpg", d["w_in"][kc * 128:(kc + 1) * 128, 1536 + hf * 1280:1536 + (hf + 1) * 1280], stage[hf][:], "wstb%d" % hf, eng=("dve" if hf == 0 else "pool"))
        for kc in range(4):
            load_cast(k, wau[:, kc, :], "wau", d["w_au"][kc * 128:(kc + 1) * 128, :], stage[kc % 2][:, 0:D], "wstb%d" % (kc % 2), eng="pool")
        for kc in range(4):
            load_cast(k, wpu[:, kc, :], "wpu", d["w_pu"][kc * 128:(kc + 1) * 128, :], stage[kc % 2][:, 0:D], "wstb%d" % (kc % 2), eng="dve")
        for kc in range(8):
            load_cast(k, wout[:, kc, :], "wout", d["w_out"][kc * 128:(kc + 1) * 128, :], stage[kc % 2][:, 0:D], "wstb%d" % (kc % 2), eng=("dve" if kc % 2 == 0 else "pool"))
        for g in range(4):
            load_cast(k, wmix[:, g, :], "wmix", d["w_mix"][g], stage[g % 2][:, 0:128], "wstb%d" % (g % 2), eng="dve")
        S.dma(lambda e: e.dma_start(out=bmix[:], in_=d["b_mix"]), writes=["bmixc"])
        S.dma(lambda e: e.dma_start(out=pscale[:], in_=d["pscale"]), writes=["pscalec"])
        S.dma(lambda e: e.dma_start(out=invc[:], in_=d["invcnt16"]), writes=["invc"])
        S.dve(lambda e: e.memset(pbuf[0][:, :, 0:16], 0.0), writes=["pbuf0"])

        acol = k.modcols[:, 0:8]
        bcol = k.modcols[:, 8:16]
        winsrc = [s2, s4, s8, s16]
        winname = ["s2", "s4", "s8", "s16"]
        for b in range(k.opts.get("nb1b", NBLK)):
            pb = pbuf[b % 2]
            pbn = "pbuf%d" % (b % 2)
            norm_block(k, T2, d["x"], b * BLK, acol, bcol, hT, "hTB", "B")
            if b > 0:
                pprev = pbuf[(b - 1) % 2]
                S.dve(lambda e, pb=pb, pprev=pprev: e.tensor_copy(out=pb[:, :, 0:16], in_=pprev[:, :, 256:272]), reads=["pbuf%d" % ((b - 1) % 2)], writes=[pbn])
            for g in range(4):
                pps = k.bank[2 + g % 2][:, 0:256]
                ppn = "bank%d" % (2 + g % 2)
                for kc in range(8):
                    S.pe(lambda e, g=g, kc=kc, pps=pps: e.matmul(pps, lhsT=wpg[:, kc, g * 128:(g + 1) * 128], rhs=hT[:, kc, :], start=(kc == 0), stop=(kc == 7)),
                         reads=["wpg", "hTB"], writes=[ppn])
                S.act(lambda e, g=g, pb=pb, pps=pps: e.copy(out=pb[:, g, 16:272], in_=pps), reads=[ppn], writes=[pbn])
            S.dve(lambda e, pb=pb: e.tensor_tensor(out=s2[:, :, 1:272], in0=pb[:, :, 1:272], in1=pb[:, :, 0:271], op=ALU.add), reads=[pbn], writes=["s2"])
            S.pool(lambda e: e.tensor_tensor(out=s4[:, 1:4, 3:272], in0=s2[:, 1:4, 3:272], in1=s2[:, 1:4, 1:270], op=ALU.add), reads=["s2"], writes=["s4"])
            S.dve(lambda e: e.tensor_tensor(out=s8[:, 2:4, 7:272], in0=s4[:, 2:4, 7:272], in1=s4[:, 2:4, 3:268], op=ALU.add), reads=["s4"], writes=["s8"])
            S.pool(lambda e: e.tensor_tensor(out=s16[:, 3:4, 15:272], in0=s8[:, 3:4, 15:272], in1=s8[:, 3:4, 7:264], op=ALU.add), reads=["s8"], writes=["s16"])
            for g in range(4):
                w = float(2 ** (g + 1))
                S.dve(lambda e, g=g, w=w, pb=pb: e.scalar_tensor_tensor(out=pin[:, g, :], in0=winsrc[g][:, g, 16:272], scalar=1.0 / w, in1=pb[:, g, 16:272], op0=ALU.mult, op1=ALU.subtract),
                      reads=[winname[g], pbn], writes=["pin"])
                if b == 0:
                    S.dve(lambda e, g=g: e.tensor_tensor(out=t16[:], in0=winsrc[g][:, g, 16:32], in1=invc[:, g, :], op=ALU.mult), reads=[winname[g], "invc"], writes=["t16"])
                    S.dve(lambda e, g=g, pb=pb: e.tensor_tensor(out=pin[:, g, 0:16], in0=t16[:], in1=pb[:, g, 16:32], op=ALU.subtract), reads=["t16", pbn], writes=["pin"])
            for g in range(4):
                mps = k.bank[2 + g % 2][:, 0:256]
                mpn = "bank%d" % (2 + g % 2)
                S.pe(lambda e, g=g, mps=mps: e.matmul(mps, lhsT=wmix[:, g, :], rhs=pin[:, g, :], start=True, stop=True), reads=["wmix", "pin"], writes=[mpn])
                S.dve(lambda e, g=g, mps=mps: e.tensor_scalar(out=poolT[:, g, :], in0=mps, scalar1=bmix[:, g:g + 1], scalar2=pscale[:, g:g + 1], op0=ALU.add, op1=ALU.mult),
                      reads=[mpn, "bmixc", "pscalec"], writes=["poolT"])
            for dc in range(8):
                st_ = dc % 2
                bA = k.bank[4 + 2 * st_]
                bB = k.bank[5 + 2 * st_]
                nA = "bank%d" % (4 + 2 * st_)
                nB = "bank%d" % (5 + 2 * st_)
                aups, pups = bA[:, 0:256], bA[:, 256:512]
                gaps, gbps = bB[:, 0:256], bB[:, 256:512]
                for kc in range(4):
                    S.pe(lambda e, dc=dc, kc=kc, aups=aups, b=b: e.matmul(aups, lhsT=wau[:, kc, dc * 128:(dc + 1) * 128], rhs=k.attnT[:, kc, b * BLK:(b + 1) * BLK], start=(kc == 0), stop=(kc == 3)),
                         reads=["wau", "attnT_%d" % b], writes=[nA])
                for kc in range(4):
                    S.pe(lambda e, dc=dc, kc=kc, pups=pups: e.matmul(pups, lhsT=wpu[:, kc, dc * 128:(dc + 1) * 128], rhs=poolT[:, kc, :], start=(kc == 0), stop=(kc == 3)),
                         reads=["wpu", "poolT"], writes=[nA])
                for kc in range(8):
                    S.pe(lambda e, dc=dc, kc=kc, gaps=gaps: e.matmul(gaps, lhsT=wpg[:, kc, 512 + dc * 128:512 + (dc + 1) * 128], rhs=hT[:, kc, :], start=(kc == 0), stop=(kc == 7)),
                         reads=["wpg", "hTB"], writes=[nB])
                for kc in range(8):
                    S.pe(lambda e, dc=dc, kc=kc, gbps=gbps: e.matmul(gbps, lhsT=wpg[:, kc, 1536 + dc * 128:1536 + (dc + 1) * 128], rhs=hT[:, kc, :], start=(kc == 0), stop=(kc == 7)),
                         reads=["wpg", "hTB"], writes=[nB])
                S.act(lambda e, st_=st_, gaps=gaps: e.activation(out=sga[st_][:], in_=gaps, func=AF.Sigmoid), reads=[nB], writes=["sga%d" % st_])
                S.act(lambda e, st_=st_, gbps=gbps: e.activation(out=sgb[st_][:], in_=gbps, func=AF.Sigmoid), reads=[nB], writes=["sgb%d" % st_])
                S.dve(lambda e, st_=st_, aups=aups: e.tensor_tensor(out=m1[:], in0=sga[st_][:], in1=aups, op=ALU.mult), reads=["sga%d" % st_, nA], writes=["m1"])
                S.dve(lambda e, st_=st_, pups=pups: e.tensor_tensor(out=m2[:], in0=sgb[st_][:], in1=pups, op=ALU.mult), reads=["sgb%d" % st_, nA], writes=["m2"])
                S.pool(lambda e, dc=dc: e.tensor_tensor(out=mergedT[:, dc, :], in0=m1[:], in1=m2[:], op=ALU.add), reads=["m1", "m2"], writes=["mergedT"])
                if k.opts.get("u_in_1b", True) and u_next[0] < 128:
                    ui_ = u_next[0]
                    if ui_ == 0:
                        u_load(k, S, 0, u_ut[0], u_vt[0], ("u_ut0", "u_vt0"))
                    if ui_ + 1 < 128:
                        un_ = (ui_ + 1) % 2
                        u_load(k, S, ui_ + 1, u_ut[un_], u_vt[un_], ("u_ut%d" % un_, "u_vt%d" % un_))
                    up_ = ui_ % 2
                    u_proc(k, S, ui_, u_ut[up_], u_vt[up_], u_utc, u_vbf, (0, 1), ("u_ut%d" % up_, "u_vt%d" % up_, "u_utc", "u_vbf"))
                    u_next[0] += 1
            for tt in range(2):
                xo = x1t[tt]
                xon = "x1t%d" % tt
                for dh in range(2):
                    yps = k.bank[2 + dh]
                    ypn = "bank%d" % (2 + dh)
                    for kc in range(8):
                        S.pe(lambda e, tt=tt, dh=dh, kc=kc, yps=yps: e.matmul(yps[:, :], lhsT=mergedT[:, kc, tt * 128:(tt + 1) * 128], rhs=wout[:, kc, dh * 512:(dh + 1) * 512], start=(kc == 0), stop=(kc == 7)),
                             reads=["mergedT", "wout"], writes=[ypn])
                    S.dve(lambda e, dh=dh, xo=xo, yps=yps: e.tensor_tensor(out=xo[:, dh * 512:(dh + 1) * 512], in0=yps[:, :], in1=gt1b[:, dh * 512:(dh + 1) * 512], op=ALU.mult),
                          reads=[ypn, "gt1b"], writes=[xon])
                S.pool(lambda e, xo=xo, tt=tt: e.tensor_tensor(out=xo[:], in0=xo[:], in1=T2["xt"][tt][:], op=ALU.add), reads=[xon, "xtB%d" % tt], writes=[xon])
                r0 = b * BLK + tt * 128
                S.dma(lambda e, xo=xo, r0=r0: e.dma_start(out=d["x1s"][r0:r0 + 128, :], in_=xo[:]), reads=[xon], writes=["x1s_%d_%d" % (b, tt)])
                if "1b" in k.dbg:
                    if "x1" not in k.dbg_out:
                        k.dbg_tensor("x1", [SEQ, D])
                    ox = k.dbg_out["x1"]
                    S.dma(lambda e, xo=xo, r0=r0, ox=ox: e.dma_start(out=ox[r0:r0 + 128, :], in_=xo[:]), reads=[xon])
        if "1b" in k.dbg:
            pass


def phase_2(k):
    nc, S, d = k.nc, k.S, k.d
    with contextlib.ExitStack() as st:
        T = lambda n, s, dt=F32: k.T(n, s, dt, st)
        wq = T("wq", [128, 8, 2048], BF16)
        gt2b = T("gt2b", [128, D])
        gfb = T("gfb", [128, D])
        S.dma(lambda e: e.dma_start(out=gt2b[:], in_=d["bcs"][1]), reads=["bcs1"], writes=["gt2b"])
        S.dma(lambda e: e.dma_start(out=gfb[:], in_=d["bcs"][2]), reads=["bcs2"], writes=["gfb"])
        keysT = T("keysT", [128, 2, 128])
        kst = T("kst", [128, 128])
        iota128 = T("iota128", [128, 128])
        iota16 = T("iota16", [128, 16])
        T2 = dict(xt=[T("xtC0", [128, D]), T("xtC1", [128, D])], junk=T("junkC", [128, D], BF16), ss=T("ssC", [128, 2]), xn=T("xnC", [128, D]), tmp=T("ntmpC", [128, 4, 128]))
        stage = [T2["xt"][0], T2["xt"][1]]
        h2Ts = [T("h2T0", [128, 8, BLK], BF16), T("h2T1", [128, 8, BLK], BF16)]
        TS = 4
        NR = 8
        Rbuf = T("Rbuf", [128, 2 * NR, TS * 128], BF16)
        Rn = ["R1_%d" % i for i in range(NR)] + ["R2_%d" % i for i in range(NR)]
        R1 = [Rbuf[:, i, :].rearrange("p (t c) -> p t c", c=128) for i in range(NR)]
        R2 = [Rbuf[:, NR + i, :].rearrange("p (t c) -> p t c", c=128) for i in range(NR)]
        qpT = Rbuf[:].rearrange("p a b -> p (a b)").bitcast(F32).rearrange("p (m t) -> p m t", t=BLK)
        s_sb = T("s_sb", [128, 16, 128])
        s_sb2 = T("s_sb2", [128, 16, 128])
        vals = T("vals", [128, 16, 16])
        idxu = T("idxu", [128, 16, 16], U32)
        idxf = T("idxf", [128, 16, 16])
        cand = s_sb[:].rearrange("p m n -> p (m n)").rearrange("p (h c) -> p h c", c=256)
        cand2 = s_sb2[:].rearrange("p m n -> p (m n)").rearrange("p (h c) -> p h c", c=256)
        tops = T("tops", [128, 8, 16])
        flatu = T("flatu", [128, 8, 16], U32)
        aidxu = T("aidxu", [128, 8, 16], U32)
        bidxu = T("bidxu", [128, 8, 16], U32)
        aidx = T("aidx", [128, 8, 16])
        bidx = T("bidx", [128, 8, 16])
        gate = T("gate", [128, 8, 16])
        gsum = T("gsum", [128, 8])
        e1f = T("e1f", [128, 128])
        e2f = T("e2f", [128, 128])
        e1T = T("e1T", [128, BLK])
        e2T = T("e2T", [128, BLK])
        gateT = T("gateT", [128, BLK])
        GT = T("GT", [128, BLK, 128], BF16)
        NI = 2
        NSB = 3
        utb = [T("utb%d" % i, [128, NI, 8, 128], BF16) for i in range(NSB)]
        vb = [T("vb%d" % i, [128, NI, D], BF16) for i in range(NSB)]
        gl = [T("gl%d" % i, [128, BLK], BF16) for i in range(2)]
        actT = [T("actT%d" % i, [128, BLK], BF16) for i in range(2)]
        x2 = T("x2", [128, D])
        ss2 = T("ss2", [128, 1])
        ot = [s_sb[:].rearrange("p m n -> p (m n)")[:, 0:D], s_sb2[:].rearrange("p m n -> p (m n)")[:, 0:D]]
        otn = ["s_sb", "s_sb2"]

        for kc in range(8):
            for hf in range(2):
                load_cast(k, wq[:, kc, hf * 1024:(hf + 1) * 1024], "wq", d["w_pq"][kc * 128:(kc + 1) * 128, hf * 1024:(hf + 1) * 1024], stage[hf][:], "xtC%d" % hf, eng=("dve" if hf == 0 else "pool"))
        S.dma(lambda e: e.dma_start(out=iota128[:], in_=d["iota128"]), writes=["iota128"])
        S.dma(lambda e: e.dma_start(out=iota16[:], in_=d["iota16"]), writes=["iota16"])
        iota_bf = T("iota_bf", [128, 128], BF16)
        S.dve(lambda e: e.tensor_copy(out=iota_bf[:], in_=iota128[:]), reads=["iota128"], writes=["iota_bf"])
        for p in range(2):
            S.dma(lambda e, p=p: e.dma_start(out=kst[:], in_=d["keys"][p]), writes=["kst"])
            S.pe(lambda e: e.transpose(k.bank[7][:, 0:128], kst[:], k.ident[:]), reads=["kst", "ident"], writes=["bank7"])
            S.dve(lambda e, p=p: e.tensor_copy(out=keysT[:, p, :], in_=k.bank[7][:, 0:128]), reads=["bank7"], writes=["keysT"])

        acol = k.modcols[:, 16:24]
        bcol = k.modcols[:, 24:32]

        def prelude(S, g):
            h2T = h2Ts[g % 2]
            hn = "h2T%d" % (g % 2)
            norm_block(k, T2, d["x1s"], g * BLK, acol, bcol, h2T, hn, "C", src_deps=["x1s_%d_%d" % (g, tt_) for tt_ in range(2)], S=S, banks=(6, 7))
            for m in range(16):
                bi = 6 + m % 2
                qps = k.bank[bi][:, 0:256]
                qpn = "bank%d" % bi
                for kc in range(8):
                    S.pe(lambda e, m=m, kc=kc, qps=qps: e.matmul(qps, lhsT=wq[:, kc, m * 128:(m + 1) * 128], rhs=h2T[:, kc, :], start=(kc == 0), stop=(kc == 7)),
                         reads=["wq", hn], writes=[qpn])
                if m % 2 == 0:
                    S.act(lambda e, m=m, qps=qps: e.copy(out=qpT[:, m, :], in_=qps), reads=[qpn], writes=Rn)
                else:
                    S.dve(lambda e, m=m, qps=qps: e.tensor_copy(out=qpT[:, m, :], in_=qps), reads=[qpn], writes=Rn)
            for tt in range(2):
                for mg in range(4):
                    sps = k.bank[7]
                    for mm in range(4):
                        m = mg * 4 + mm
                        S.pe(lambda e, m=m, mm=mm, tt=tt, sps=sps: e.matmul(sps[:, mm * 128:(mm + 1) * 128], lhsT=qpT[:, m, tt * 128:(tt + 1) * 128], rhs=keysT[:, m % 2, :], start=True, stop=True),
                             reads=Rn + ["keysT"], writes=["bank7"])
                    S.act(lambda e, mg=mg, sps=sps: e.copy(out=s_sb[:, mg * 4:(mg + 1) * 4, :], in_=sps[:, :].rearrange("p (a b) -> p a b", b=128)), reads=["bank7"], writes=["s_sb"])
                for m in range(16):
                    S.dve(lambda e, m=m: e.max(out=vals[:, m, 0:8], in_=s_sb[:, m, :]), reads=["s_sb"], writes=["vals"])
                    S.dve(lambda e, m=m: e.max_index(out=idxu[:, m, 0:8], in_max=vals[:, m, 0:8], in_values=s_sb[:, m, :]), reads=["s_sb", "vals"], writes=["idxu"])
                    S.dve(lambda e, m=m: e.match_replace(out=s_sb2[:, m, :], in_to_replace=vals[:, m, 0:8], in_values=s_sb[:, m, :], imm_value=-1e30), reads=["s_sb", "vals"], writes=["s_sb2"])
                    S.dve(lambda e, m=m: e.max(out=vals[:, m, 8:16], in_=s_sb2[:, m, :]), reads=["s_sb2"], writes=["vals"])
                    S.dve(lambda e, m=m: e.max_index(out=idxu[:, m, 8:16], in_max=vals[:, m, 8:16], in_values=s_sb2[:, m, :]), reads=["s_sb2", "vals"], writes=["idxu"])
                S.pool(lambda e: e.tensor_copy(out=idxf[:], in_=idxu[:]), reads=["idxu"], writes=["idxf"])
                v4 = vals[:].rearrange("p (h two) k -> p h two k", two=2)
                S.dve(lambda e, v4=v4: e.tensor_tensor(out=cand[:].rearrange("p h (a b) -> p h a b", b=16), in0=v4[:, :, 0, :].unsqueeze(3).to_broadcast([128, 8, 16, 16]),
                                                      in1=v4[:, :, 1, :].unsqueeze(2).to_broadcast([128, 8, 16, 16]), op=ALU.add), reads=["vals"], writes=["s_sb"])
                for h in range(8):
                    S.dve(lambda e, h=h: e.max(out=tops[:, h, 0:8], in_=cand[:, h, :]), reads=["s_sb"], writes=["tops"])
                    S.dve(lambda e, h=h: e.max_index(out=flatu[:, h, 0:8], in_max=tops[:, h, 0:8], in_values=cand[:, h, :]), reads=["s_sb", "tops"], writes=["flatu"])
                    S.dve(lambda e, h=h: e.match_replace(out=cand2[:, h, :], in_to_replace=tops[:, h, 0:8], in_values=cand[:, h, :], imm_value=-1e30), reads=["s_sb", "tops"], writes=["s_sb2"])
                    S.dve(lambda e, h=h: e.max(out=tops[:, h, 8:16], in_=cand2[:, h, :]), reads=["s_sb2"], writes=["tops"])
                    S.dve(lambda e, h=h: e.max_index(out=flatu[:, h, 8:16], in_max=tops[:, h, 8:16], in_values=cand2[:, h, :]), reads=["s_sb2", "tops"], writes=["flatu"])
                S.dve(lambda e: e.tensor_tensor(out=gate[:], in0=tops[:], in1=tops[:, :, 0:1].to_broadcast([128, 8, 16]), op=ALU.subtract), reads=["tops"], writes=["gate"])
                S.act(lambda e: e.activation(out=gate[:], in_=gate[:], func=AF.Exp), reads=["gate"], writes=["gate"])
                S.dve(lambda e: e.tensor_reduce(out=gsum[:], in_=gate[:], axis=AX.X, op=ALU.add), reads=["gate"], writes=["gsum"])
                S.dve(lambda e: e.reciprocal(out=gsum[:], in_=gsum[:]), reads=["gsum"], writes=["gsum"])
                S.dve(lambda e: e.tensor_tensor(out=gate[:], in0=gate[:], in1=gsum[:].unsqueeze(2).to_broadcast([128, 8, 16]), op=ALU.mult), reads=["gate", "gsum"], writes=["gate"])
                S.dve(lambda e: e.tensor_scalar(out=aidxu[:], in0=flatu[:], scalar1=4, scalar2=None, op0=ALU.logical_shift_right), reads=["flatu"], writes=["aidxu"])
                S.dve(lambda e: e.tensor_scalar(out=bidxu[:], in0=flatu[:], scalar1=15, scalar2=None, op0=ALU.bitwise_and), reads=["flatu"], writes=["bidxu"])
                S.pool(lambda e: e.tensor_copy(out=aidx[:], in_=aidxu[:]), reads=["aidxu"], writes=["aidx"])
                S.pool(lambda e: e.tensor_copy(out=bidx[:], in_=bidxu[:]), reads=["bidxu"], writes=["bidx"])
                i4 = idxf[:].rearrange("p (h two) k -> p h two k", two=2)
                eq = cand[:].rearrange("p h (a b) -> p (h a) b", b=16)
                eq4 = cand[:].rearrange("p h (a b) -> p h a b", b=16)
                for which, (src_idx, dst) in enumerate(((aidx, e1f), (bidx, e2f))):
                    S.dve(lambda e, src_idx=src_idx: e.tensor_tensor(out=eq, in0=src_idx[:].rearrange("p h k -> p (h k)").unsqueeze(2).to_broadcast([128, 128, 16]),
                                                                     in1=iota16[:].unsqueeze(1).to_broadcast([128, 128, 16]), op=ALU.is_equal), reads=["aidx", "bidx", "iota16"], writes=["s_sb"])
                    S.dve(lambda e, which=which: e.tensor_tensor(out=eq4, in0=eq4, in1=i4[:, :, which, :].unsqueeze(2).to_broadcast([128, 8, 16, 16]), op=ALU.mult), reads=["s_sb", "idxf"], writes=["s_sb"])
                    S.dve(lambda e, dst=dst: e.tensor_reduce(out=dst[:], in_=eq, axis=AX.X, op=ALU.add), reads=["s_sb"], writes=["e1f" if which == 0 else "e2f"])
                for (src, sn, dst, dn, col) in ((e1f, "e1f", e1T, "e1T", 0), (e2f, "e2f", e2T, "e2T", 1), (gate, "gate", gateT, "gateT", 2)):
                    src_ap = src[:] if src is not gate else gate[:].rearrange("p h k -> p (h k)")
                    S.pe(lambda e, src_ap=src_ap, col=col: e.transpose(k.bank[6][:, col * 128:(col + 1) * 128], src_ap, k.ident[:]), reads=[sn, "ident"], writes=["bank6"])
                    S.act(lambda e, dst=dst, col=col, tt=tt: e.copy(out=dst[:, tt * 128:(tt + 1) * 128], in_=k.bank[6][:, col * 128:(col + 1) * 128]), reads=["bank6"], writes=[dn])

        def gcon(S, g):
            for sc in range(BLK // TS):
                r1 = R1[sc % NR]; r2 = R2[sc % NR]
                n1 = "R1_%d" % (sc % NR); n2 = "R2_%d" % (sc % NR)
                for tl in range(TS):
                    tcol = sc * TS + tl
                    S.dve(lambda e, r1=r1, tl=tl, tcol=tcol: e.tensor_scalar(out=r1[:, tl, :], in0=iota_bf[:], scalar1=e1T[:, tcol:tcol + 1], scalar2=None, op0=ALU.is_equal),
                                                        reads=["iota_bf", "e1T"], writes=[n1])
                    S.dve(lambda e, r2=r2, tl=tl, tcol=tcol: e.tensor_scalar(out=r2[:, tl, :], in0=iota_bf[:], scalar1=e2T[:, tcol:tcol + 1], scalar2=gateT[:, tcol:tcol + 1], op0=ALU.is_equal, op1=ALU.mult),
                          reads=["iota_bf", "e2T", "gateT"], writes=[n2])
                for q4 in range(TS // 4):
                    gb = 4 + (sc * (TS // 4) + q4) % 2
                    gps = k.bank[gb]
                    for tl in range(4):
                        t_in = q4 * 4 + tl
                        S.pe(lambda e, gps=gps, tl=tl, t_in=t_in, r1=r1, r2=r2: e.matmul(gps[:, tl * 128:(tl + 1) * 128], lhsT=r2[:, t_in, :], rhs=r1[:, t_in, :], start=True, stop=True),
                             reads=[n1, n2], writes=["bank%d" % gb])
                    t0 = sc * TS + q4 * 4
                    S.act(lambda e, gps=gps, t0=t0: e.copy(out=GT[:, t0:t0 + 4, :], in_=gps[:, :].rearrange("p (a b) -> p a b", b=128)), reads=["bank%d" % gb], writes=["GT"])

        def main_load(S, g, ip):
            sb = ip % NSB
            S.dma(lambda e, ip=ip, sb=sb: e.dma_start(out=utb[sb][:].rearrange("p n a b -> p (n a b)"), in_=d["uts"][:, NI * ip:NI * (ip + 1), :].rearrange("p n c -> p (n c)")),
                  reads=["uts%d" % ip], writes=["utb%d" % sb])
            S.dma(lambda e, ip=ip, sb=sb: e.dma_start(out=vb[sb][:].rearrange("p n c -> p (n c)"), in_=d["vs"][:, NI * ip:NI * (ip + 1), :].rearrange("p n c -> p (n c)")),
                  reads=["vs%d" % ip], writes=["vb%d" % sb])

        def main_A(S, g, i):
            h2T = h2Ts[g % 2]
            hn = "h2T%d" % (g % 2)
            sb = (i // NI) % NSB
            ii = i % NI
            bi = 4 + i % 2
            aps = k.bank[bi][:, 0:256]
            for kc in range(8):
                S.pe(lambda e, sb=sb, ii=ii, kc=kc, aps=aps: e.matmul(aps, lhsT=utb[sb][:, ii, kc, :], rhs=h2T[:, kc, :], start=(kc == 0), stop=(kc == 7)), reads=["utb%d" % sb, hn], writes=["bank%d" % bi])

        def main_EV(S, g, i):
            sb = (i // NI) % NSB
            ii = i % NI
            bi = 4 + i % 2
            aps = k.bank[bi][:, 0:256]
            S.act(lambda e, i=i, aps=aps: e.activation(out=gl[i % 2][:], in_=aps, func=AF.Gelu_apprx_tanh), reads=["bank%d" % bi], writes=["gl%d" % (i % 2)])
            S.pool(lambda e, i=i: e.tensor_tensor(out=actT[i % 2][:], in0=gl[i % 2][:], in1=GT[:, :, i], op=ALU.mult), reads=["gl%d" % (i % 2), "GT"], writes=["actT%d" % (i % 2)])
            for tt in range(2):
                for dh in range(2):
                    yb = tt * 2 + dh
                    S.pe(lambda e, i=i, ii=ii, tt=tt, dh=dh, yb=yb, sb=sb: e.matmul(k.bank[yb][:, :], lhsT=actT[i % 2][:, tt * 128:(tt + 1) * 128], rhs=vb[sb][:, ii, dh * 512:(dh + 1) * 512],
                                                                                 start=(i == 0), stop=(i == 127)), reads=["actT%d" % (i % 2), "vb%d" % sb], writes=["bank%d" % yb])

        def epilogue(S, g):
            for tt in range(2):
                o = ot[tt]
                on = otn[tt]
                r0 = g * BLK + tt * 128
                S.dma(lambda e, o=o, r0=r0: e.dma_start(out=o[:], in_=d["x1s"][r0:r0 + 128, :]), reads=["x1s_%d_%d" % (g, tt)], writes=[on])
                for dh in range(2):
                    yb = tt * 2 + dh
                    S.dve(lambda e, dh=dh, yb=yb: e.tensor_tensor(out=x2[:, dh * 512:(dh + 1) * 512], in0=k.bank[yb][:, :], in1=gt2b[:, dh * 512:(dh + 1) * 512], op=ALU.mult),
                          reads=["bank%d" % yb, "gt2b"], writes=["x2"])
                S.dve(lambda e, o=o: e.tensor_tensor(out=x2[:], in0=x2[:], in1=o[:], op=ALU.add), reads=["x2", on], writes=["x2"])
                S.act(lambda e, o=o: e.activation(out=o[:], in_=x2[:], func=AF.Square, accum_out=ss2[:]), reads=["x2"], writes=[on, "ss2"])
                S.dve(lambda e: e.tensor_scalar(out=ss2[:], in0=ss2[:], scalar1=1.0 / D, scalar2=EPS, op0=ALU.mult, op1=ALU.add), reads=["ss2"], writes=["ss2"])
                S.act(lambda e: e.activation(out=ss2[:], in_=ss2[:], func=AF.Sqrt), reads=["ss2"], writes=["ss2"])
                S.dve(lambda e: e.reciprocal(out=ss2[:], in_=ss2[:]), reads=["ss2"], writes=["ss2"])
                S.dve(lambda e, o=o: e.scalar_tensor_tensor(out=o[:], in0=x2[:], scalar=ss2[:, 0:1], in1=gfb[:], op0=ALU.mult, op1=ALU.mult), reads=["x2", "ss2", "gfb"], writes=[on])
                S.dma(lambda e, o=o, r0=r0: e.dma_start(out=d["out"][r0:r0 + 128, :], in_=o[:]), reads=[on], writes=["out_%d" % r0])

        ngrp = k.opts.get("ngrp", NBLK)
        prelude(S, 0)
        for g in range(ngrp):
            gcon(S, g)
            rec = Rec()
            if g + 1 < ngrp:
                prelude(rec, g + 1)
            per = (len(rec.l) + 127) // 128
            main_load(S, g, 0)
            main_load(S, g, 1)
            main_A(S, g, 0)
            for i in range(128):
                if (i + 1) % NI == 0 and (i + 1) // NI + 1 < 128 // NI:
                    main_load(S, g, (i + 1) // NI + 1)
                if i + 1 < 128:
                    main_A(S, g, i + 1)
                main_EV(S, g, i)
                rec.replay(S, per)
            rec.replay(S)
            epilogue(S, g)


_CACHE = {}


def make_in_maps(inputs):
    f = lambda a: np.ascontiguousarray(np.asarray(a, dtype=np.float32))
    hc = host_consts()
    common = {
        "w_ada": f(inputs["w_ada"][0]), "b_ada": f(inputs["b_ada"][0]).reshape(1, -1),
        "g1": f(inputs["g_norm1"][0]).reshape(1, -1), "g2": f(inputs["g_norm2"][0]).reshape(1, -1), "gf": f(inputs["g_final"]).reshape(1, -1),
        "w_in": f(inputs["w_in"][0]), "w_au": f(inputs["w_attn_up"][0]), "w_mix": f(inputs["w_pool_mix"][0]),
        "b_mix": f(np.asarray(inputs["b_pool_mix"][0]).T), "pscale": f(np.asarray(inputs["pool_scale"][0]).reshape(4, 128).T),
        "w_pu": f(inputs["w_pool_up"][0]), "w_out": f(inputs["w_out"][0]), "w_pq": f(inputs["w_peer_q"][0]),
        "keys": f(inputs["peer_sub_keys"][0]), "peer_u": f(inputs["peer_u"][0]), "peer_v": f(inputs["peer_v"][0]),
    }
    common.update(hc)
    maps = []
    x = np.asarray(inputs["x"], dtype=np.float32)
    c = np.asarray(inputs["c"], dtype=np.float32)
    for b in range(x.shape[0]):
        m = dict(common)
        m["x"] = np.ascontiguousarray(x[b])
        m["c"] = np.ascontiguousarray(c[b].reshape(8, 128).T)
        maps.append(m)
    return maps


def kernel(**inputs):
    if "nc" not in _CACHE:
        _CACHE["nc"] = build()[0]
    nc = _CACHE["nc"]
    maps = make_in_maps(inputs)
    res = run_bass_kernel_spmd(nc, maps, core_ids=list(range(8)))
    return np.stack([np.asarray(r["out"], dtype=np.float32) for r in res.results], axis=0)
```

```python
import contextlib
import numpy as np
import ml_dtypes
import concourse.bass as bass
import concourse.mybir as mybir
from concourse.bass_utils import run_bass_kernel_spmd

F32 = mybir.dt.float32
BF16 = mybir.dt.bfloat16
U32 = mybir.dt.uint32
AF = mybir.ActivationFunctionType
ALU = mybir.AluOpType
AX = mybir.AxisListType

SEQ = 4096
D = 1024
NBLK = 16
BLK = 256
NEXP_CH = 128
EPS = 1e-6
NEG = -30000.0

ENGS = ["pe", "act", "dve", "pool", "sp"]
ENGMAP = {"pe": "tensor", "act": "scalar", "dve": "vector", "pool": "gpsimd", "sp": "sync"}


class Sched:
    LIM = 30000

    def __init__(self, nc):
        self.nc = nc
        self.ops = []
        self.last_write = {}
        self.readers = {}
        self.seen = set()
        self.bar = set()
        self.lastop = {}
        self.dma_ops = []

    def add(self, eng, fn, reads=(), writes=(), dma=False):
        deps = set()
        reads = list(reads)
        writes = list(writes)
        for b in list(reads):
            if b.startswith("bank") and b not in writes:
                writes.append(b)
        for b in list(reads) + list(writes):
            if b not in self.seen:
                self.seen.add(b)
                deps |= self.bar
        for b in reads:
            if b in self.last_write:
                deps.add(self.last_write[b])
        for b in writes:
            if b in self.last_write:
                deps.add(self.last_write[b])
            deps.update(self.readers.get(b, ()))
        idx = len(self.ops)
        self.ops.append(dict(eng=eng, fn=fn, deps=deps, dma=dma, sig=dma, sem=None, val=None))
        for b in reads:
            self.readers.setdefault(b, []).append(idx)
        for b in writes:
            self.last_write[b] = idx
            self.readers[b] = []
        self.lastop[(eng, dma)] = idx
        if dma:
            self.dma_ops.append(idx)
        return idx

    def barrier(self):
        self.bar = set(v for kk, v in self.lastop.items() if not kk[1]) | set(self.dma_ops[-self.NDMA:])
        self.seen = set()

    def pe(self, fn, reads=(), writes=()):
        return self.add("pe", fn, reads, writes)

    def act(self, fn, reads=(), writes=()):
        return self.add("act", fn, reads, writes)

    def dve(self, fn, reads=(), writes=()):
        return self.add("dve", fn, reads, writes)

    def pool(self, fn, reads=(), writes=()):
        return self.add("pool", fn, reads, writes)

    def dma(self, fn, reads=(), writes=(), q="sp"):
        return self.add(q, fn, reads, writes, dma=True)

    @staticmethod
    def _skip(src, op):
        return src["eng"] == "pe" and op["eng"] == "pe" and not src["dma"] and not op["dma"]

    NDMA = 32

    def emit(self, final_wait_eng="sp"):
        nc = self.nc
        ops = self.ops
        for op in ops:
            for d in op["deps"]:
                if not self._skip(ops[d], op):
                    ops[d]["sig"] = True
        cnt = {}
        epoch = {}
        semkeys = []
        ndma = 0
        for op in ops:
            if op["dma"]:
                slot = ndma % self.NDMA
                op["sem"] = ("dma", slot)
                op["val"] = 16 * (ndma // self.NDMA + 1)
                op["ev"] = (0, op["val"])
                ndma += 1
                continue
            if not op["sig"]:
                continue
            key = op["eng"]
            if key not in cnt:
                cnt[key] = 0
                epoch[key] = 0
                semkeys.append((key, 0))
            if cnt[key] + 1 > self.LIM:
                epoch[key] += 1
                cnt[key] = 0
                semkeys.append((key, epoch[key]))
            cnt[key] += 1
            op["sem"] = (key, epoch[key])
            op["val"] = cnt[key]
            op["ev"] = (epoch[key], cnt[key])
        for slot in range(min(ndma, self.NDMA)):
            semkeys.append(("dma", slot))
        with contextlib.ExitStack() as st:
            sems = {}
            for k in semkeys:
                sems[k] = st.enter_context(nc.semaphore("s_%s_%d" % (k[0], k[1])))
            block = st.enter_context(nc.Block())

            def make(engname):
                def body(e):
                    waited = {}

                    def wait(wk, ev):
                        if waited.get(wk, (-1, -1)) >= ev:
                            return
                        semk = wk if isinstance(wk, tuple) else (wk, ev[0])
                        e.wait_ge(sems[semk], ev[1])
                        waited[wk] = ev

                    for op in ops:
                        if op["eng"] != engname:
                            continue
                        need = {}
                        for d in op["deps"]:
                            src = ops[d]
                            if not src["sig"] or self._skip(src, op):
                                continue
                            wk = src["sem"] if src["dma"] else src["eng"]
                            if need.get(wk, (-1, -1)) < src["ev"]:
                                need[wk] = src["ev"]
                        if op["dma"] and op["val"] > 16:
                            wk = op["sem"]
                            ev = (0, op["val"] - 16)
                            if need.get(wk, (-1, -1)) < ev:
                                need[wk] = ev
                        for wk in sorted(need, key=str):
                            wait(wk, need[wk])
                        ins = op["fn"](e)
                        if op["dma"]:
                            ins.then_inc(sems[op["sem"]], 16)
                        elif op["sig"]:
                            ins.then_inc(sems[op["sem"]], 1)
                    if engname == final_wait_eng:
                        last = {}
                        for op in ops:
                            if op["dma"]:
                                last[op["sem"]] = op["ev"]
                        for wk in sorted(last, key=str):
                            wait(wk, last[wk])
                return body

            used = set(op["eng"] for op in ops) | {final_wait_eng}
            for engname in ENGS:
                if engname in used:
                    getattr(block, ENGMAP[engname])(make(engname))
        return len(semkeys)


class Rec:
    def __init__(self):
        self.l = []

    def pe(self, *a, **kw):
        self.l.append(("pe", a, kw))

    def act(self, *a, **kw):
        self.l.append(("act", a, kw))

    def dve(self, *a, **kw):
        self.l.append(("dve", a, kw))

    def pool(self, *a, **kw):
        self.l.append(("pool", a, kw))

    def dma(self, *a, **kw):
        self.l.append(("dma", a, kw))

    def replay(self, S, n=None):
        n = len(self.l) if n is None else min(n, len(self.l))
        for _ in range(n):
            m, a, kw = self.l.pop(0)
            getattr(S, m)(*a, **kw)


def host_consts():
    c = {}
    c["ident_f"] = np.eye(128, dtype=np.float32)
    slopes = 2.0 ** (-(np.arange(8) + 1.0))
    kc = np.zeros((18, SEQ), np.float32)
    kpos = np.arange(SEQ)
    for n in range(16):
        kc[n, kpos // BLK == n] = 1.0
    kc[16, :] = 1.0
    kc[17, :] = np.where((kpos % BLK) >= 128, 128.0, 0.0)
    c["kconst"] = kc.astype(ml_dtypes.bfloat16)
    qal = np.zeros((2, 8, BLK), np.float32)
    qi = np.arange(BLK)
    for h in range(8):
        qal[0, h, :] = -slopes[h] * qi
        qal[1, h, :] = slopes[h]
    c["qal"] = qal.astype(ml_dtypes.bfloat16)
    p = np.arange(128)
    bt = np.zeros((128, 8, 16), np.float32)
    for h in range(8):
        for df in range(16):
            bt[:, h, df] = slopes[h] * (p - 256.0 * df)
    c["biasT"] = bt
    cn = np.zeros((128, 2, BLK), np.float32)
    for kh in range(2):
        ki = 128 * kh + p
        cn[:, kh, :] = np.where(ki[:, None] > qi[None, :], NEG, 0.0)
    c["causalneg"] = cn
    mb = np.zeros((128, 16, 16), np.float32)
    for b in range(16):
        mb[:, b, b:] = -1e30
    c["maskb"] = mb
    ic = np.zeros((128, 4, 16), np.float32)
    for g, w in enumerate((2, 4, 8, 16)):
        ic[:, g, :] = 1.0 / np.minimum(np.arange(16) + 1, w)
    c["invcnt16"] = ic
    c["iota128"] = np.tile(np.arange(128, dtype=np.float32)[None, :], (128, 1))
    c["iota16"] = np.tile(np.arange(16, dtype=np.float32)[None, :], (128, 1))
    return c


class K:
    pass


def build(dbg=None, phases="AU1x2", opts=None):
    nc = bass.Bass("TRN2", target_bir_lowering=False)
    k = K()
    k.nc = nc
    k.S = Sched(nc)
    k.dbg = dbg or ()
    k.opts = opts or {}
    S = k.S

    def din(name, shape, dt=F32):
        return nc.dram_tensor(name, list(shape), dt, kind="ExternalInput").ap()

    d = {}
    d["x"] = din("x", [SEQ, D])
    d["c"] = din("c", [128, 8])
    d["w_ada"] = din("w_ada", [D, 6 * D])
    d["b_ada"] = din("b_ada", [1, 6 * D])
    d["g1"] = din("g1", [1, D])
    d["g2"] = din("g2", [1, D])
    d["gf"] = din("gf", [1, D])
    d["w_in"] = din("w_in", [D, 4096])
    d["w_au"] = din("w_au", [512, D])
    d["w_mix"] = din("w_mix", [4, 128, 128])
    d["b_mix"] = din("b_mix", [128, 4])
    d["pscale"] = din("pscale", [128, 4])
    d["w_pu"] = din("w_pu", [512, D])
    d["w_out"] = din("w_out", [D, D])
    d["w_pq"] = din("w_pq", [D, 2048])
    d["keys"] = din("keys", [2, 128, 128])
    d["peer_u"] = din("peer_u", [16384, D])
    d["peer_v"] = din("peer_v", [16384, D])
    hc = host_consts()
    for nm, arr in hc.items():
        d[nm] = din(nm, arr.shape, BF16 if arr.dtype == ml_dtypes.bfloat16 else F32)
    d["out"] = nc.dram_tensor("out", [SEQ, D], F32, kind="ExternalOutput").ap()
    d["x1s"] = nc.dram_tensor("x1s", [SEQ, D], F32, kind="Internal").ap()
    d["uts"] = nc.dram_tensor("uts", [128, 128, 1024], BF16, kind="Internal").ap()
    d["vs"] = nc.dram_tensor("vs", [128, 128, 1024], BF16, kind="Internal").ap()
    d["bcs"] = nc.dram_tensor("bcs", [3, 128, D], F32, kind="Internal").ap()
    k.d = d
    k.dbg_out = {}

    def dbg_tensor(name, shape, dt=F32):
        k.dbg_out[name] = nc.dram_tensor("dbg_" + name, list(shape), dt, kind="ExternalOutput").ap()
        return k.dbg_out[name]

    k.dbg_tensor = dbg_tensor

    with contextlib.ExitStack() as st0:
        def T(name, shape, dt=F32, st=st0):
            return st.enter_context(nc.sbuf_tensor("t_" + name, list(shape), dt))

        k.T = T
        k.bank = [st0.enter_context(nc.psum_tensor("bank%d" % i, [128, 512], F32)) for i in range(8)]
        k.ident = T("ident", [128, 128])
        k.modcols = T("modcols", [128, 32])
        S.dma(lambda e: e.dma_start(out=k.ident[:], in_=d["ident_f"]), writes=["ident"])
        k.neghalf = T("neghalf", [128, 2])
        S.dve(lambda e: e.memset(k.neghalf[:], -0.5), writes=["neghalf"])
        if "A" in phases:
            phase_A(k)
        S.barrier()
        if "U" in phases and not k.opts.get("u_in_1b", True):
            phase_U(k)
        S.barrier()
        if "1" in phases:
            with contextlib.ExitStack() as st1:
                k.attnT = T("attnT", [128, 4, SEQ], BF16, st1)
                if "1a" in phases or "1x" in phases:
                    phase_1a(k)
                S.barrier()
                if "1b" in phases or "1x" in phases:
                    phase_1b(k)
        S.barrier()
        if "2" in phases:
            phase_2(k)
        nsem = S.emit()
    k.nsem = nsem
    return nc, k


def phase_A(k):
    nc, S, d = k.nc, k.S, k.d
    with contextlib.ExitStack() as st:
        T = lambda n, s, dt=F32: k.T(n, s, dt, st)
        ccol = T("ccol", [128, 8])
        cact = T("cact", [128, 8])
        wt = [T("wadaA", [128, 8, 512]), T("wadaB", [128, 8, 512])]
        modrow = T("modrow", [1, 6 * D])
        badar = T("badar", [1, 6 * D])
        g1r = T("g1r", [1, D])
        g2r = T("g2r", [1, D])
        gfr = T("gfr", [1, D])
        rows = T("rows", [1, 4, D])
        onesr = T("onesr", [1, 128])
        S.dma(lambda e: e.dma_start(out=ccol[:], in_=d["c"]), writes=["ccol"])
        S.act(lambda e: e.activation(out=cact[:], in_=ccol[:], func=AF.Silu), reads=["ccol"], writes=["cact"])
        S.dma(lambda e: e.dma_start(out=badar[:], in_=d["b_ada"]), writes=["badar"])
        S.dma(lambda e: e.dma_start(out=g1r[:], in_=d["g1"]), writes=["g1r"])
        S.dma(lambda e: e.dma_start(out=g2r[:], in_=d["g2"]), writes=["g2r"])
        S.dma(lambda e: e.dma_start(out=gfr[:], in_=d["gf"]), writes=["gfr"])
        S.dve(lambda e: e.memset(onesr[:], 1.0), writes=["onesr"])
        wv = d["w_ada"].rearrange("(kc p) n -> p kc n", p=128)
        for nch in range(12):
            w = wt[nch % 2]
            wn = "wada%d" % (nch % 2)
            pn = "bank%d" % (nch % 2)
            ps = k.bank[nch % 2]
            S.dma(lambda e, w=w, nch=nch: e.dma_start(out=w[:], in_=wv[:, :, nch * 512:(nch + 1) * 512]), writes=[wn])
            for kc in range(8):
                S.pe(lambda e, ps=ps, w=w, kc=kc: e.matmul(ps[0:1, :], lhsT=cact[:, kc:kc + 1], rhs=w[:, kc, :], start=(kc == 0), stop=(kc == 7)),
                     reads=["cact", wn], writes=[pn])
            S.dve(lambda e, ps=ps, nch=nch: e.tensor_tensor(out=modrow[0:1, nch * 512:(nch + 1) * 512], in0=ps[0:1, :], in1=badar[0:1, nch * 512:(nch + 1) * 512], op=ALU.add),
                  reads=[pn, "badar"], writes=["modrow"])
        S.dve(lambda e: e.scalar_tensor_tensor(out=rows[0:1, 0, :], in0=modrow[0:1, D:2 * D], scalar=1.0, in1=g1r[0:1, :], op0=ALU.add, op1=ALU.mult),
              reads=["modrow", "g1r"], writes=["rows"])
        S.dve(lambda e: e.tensor_copy(out=rows[0:1, 1, :], in_=modrow[0:1, 0:D]), reads=["modrow"], writes=["rows"])
        S.dve(lambda e: e.scalar_tensor_tensor(out=rows[0:1, 2, :], in0=modrow[0:1, 4 * D:5 * D], scalar=1.0, in1=g2r[0:1, :], op0=ALU.add, op1=ALU.mult),
              reads=["modrow", "g2r"], writes=["rows"])
        S.dve(lambda e: e.tensor_copy(out=rows[0:1, 3, :], in_=modrow[0:1, 3 * D:4 * D]), reads=["modrow"], writes=["rows"])
        colps = k.bank[2]
        for v in range(4):
            for kc in range(8):
                S.pe(lambda e, v=v, kc=kc: e.matmul(colps[:, v * 8 + kc:v * 8 + kc + 1], lhsT=rows[0:1, v, kc * 128:(kc + 1) * 128], rhs=onesr[0:1, 0:1], start=True, stop=True),
                     reads=["rows", "onesr"], writes=["bank2"])
        S.dve(lambda e: e.tensor_copy(out=k.modcols[:], in_=colps[:, 0:32]), reads=["bank2"], writes=["modcols"])
        bct = [T("bct0", [128, D]), T("bct1", [128, D]), T("bct2", [128, D])]
        srcs = [(bct[0], "bct0", modrow, 2 * D, "modrow"), (bct[1], "bct1", modrow, 5 * D, "modrow"), (bct[2], "bct2", gfr, 0, "gfr")]
        i = 0
        for dst, dn, src, off, sn in srcs:
            for half in range(2):
                bi = 3 + (i % 2)
                i += 1
                ps = k.bank[bi]
                S.pe(lambda e, ps=ps, src=src, off=off, half=half: e.matmul(ps[:, :], lhsT=onesr[0:1, :], rhs=src[0:1, off + half * 512:off + (half + 1) * 512], start=True, stop=True),
                     reads=["onesr", sn], writes=["bank%d" % bi])
                S.act(lambda e, ps=ps, dst=dst, half=half: e.copy(out=dst[:, half * 512:(half + 1) * 512], in_=ps[:, :]), reads=["bank%d" % bi], writes=[dn])
        for j in range(3):
            S.dma(lambda e, j=j: e.dma_start(out=d["bcs"][j], in_=bct[j][:]), reads=["bct%d" % j], writes=["bcs%d" % j])
        if "A" in k.dbg:
            o1 = k.dbg_tensor("modcols", [128, 32])
            o2 = k.dbg_tensor("gt1b", [128, D])
            S.dma(lambda e: e.dma_start(out=o1, in_=k.modcols[:]), reads=["modcols"])
            S.dma(lambda e: e.dma_start(out=o2, in_=bct[0][:]), reads=["bct0"])


def phase_U(k):
    nc, S, d = k.nc, k.S, k.d
    with contextlib.ExitStack() as st:
        T = lambda n, s, dt=F32: k.T(n, s, dt, st)
        ut = [T("ut0", [128, D]), T("ut1", [128, D])]
        vt = [T("vt0", [128, D]), T("vt1", [128, D])]
        utc = [T("utc0", [128, 8, 128], BF16), T("utc1", [128, 8, 128], BF16)]
        vbf = [T("vbf0", [128, D], BF16), T("vbf1", [128, D], BF16)]
        for i in range(128):
            j = i % 2
            S.dma(lambda e, i=i, j=j: e.dma_start(out=ut[j][:], in_=d["peer_u"][i * 128:(i + 1) * 128, :]), writes=["ut%d" % j])
            S.dma(lambda e, i=i, j=j: e.dma_start(out=vt[j][:], in_=d["peer_v"][i * 128:(i + 1) * 128, :]), writes=["vt%d" % j])
            for half in range(2):
                bi = 2 * j + half
                ps = k.bank[bi]
                for q in range(4):
                    kc = half * 4 + q
                    S.pe(lambda e, ps=ps, q=q, kc=kc, j=j: e.transpose(ps[:, q * 128:(q + 1) * 128], ut[j][:, kc * 128:(kc + 1) * 128], k.ident[:]),
                         reads=["ut%d" % j, "ident"], writes=["bank%d" % bi])
                fn = lambda e, ps=ps, half=half, j=j: e.tensor_copy(out=utc[j][:, half * 4:(half + 1) * 4, :], in_=ps[:, :].rearrange("p (a b) -> p a b", b=128))
                fa = lambda e, ps=ps, half=half, j=j: e.copy(out=utc[j][:, half * 4:(half + 1) * 4, :], in_=ps[:, :].rearrange("p (a b) -> p a b", b=128))
                if half == 0:
                    S.dve(fn, reads=["bank%d" % bi], writes=["utc%d" % j])
                else:
                    S.act(fa, reads=["bank%d" % bi], writes=["utc%d" % j])
            S.dma(lambda e, i=i, j=j: e.dma_start(out=d["uts"][:, i, :], in_=utc[j][:].rearrange("p a b -> p (a b)")), reads=["utc%d" % j], writes=["uts%d" % (i // 2)])
            S.pool(lambda e, j=j: e.tensor_copy(out=vbf[j][:], in_=vt[j][:]), reads=["vt%d" % j], writes=["vbf%d" % j])
            S.dma(lambda e, i=i, j=j: e.dma_start(out=d["vs"][:, i, :], in_=vbf[j][:]), reads=["vbf%d" % j], writes=["vs%d" % (i // 2)])


def u_load(k, S, i, ut, vt, names):
    d = k.d
    nut, nvt = names[0], names[1]
    S.dma(lambda e: e.dma_start(out=ut[:], in_=d["peer_u"][i * 128:(i + 1) * 128, :]), writes=[nut])
    S.dma(lambda e: e.dma_start(out=vt[:], in_=d["peer_v"][i * 128:(i + 1) * 128, :]), writes=[nvt])


def u_proc(k, S, i, ut, vt, utc, vbf, banks, names):
    d = k.d
    nut, nvt, nutc, nvbf = names
    for half in range(2):
        bi = banks[half]
        ps = k.bank[bi]
        for q in range(4):
            kc = half * 4 + q
            S.pe(lambda e, ps=ps, q=q, kc=kc: e.transpose(ps[:, q * 128:(q + 1) * 128], ut[:, kc * 128:(kc + 1) * 128], k.ident[:]), reads=[nut, "ident"], writes=["bank%d" % bi])
        if half == 0:
            S.dve(lambda e, ps=ps: e.tensor_copy(out=utc[:, 0:4, :], in_=ps[:, :].rearrange("p (a b) -> p a b", b=128)), reads=["bank%d" % bi], writes=[nutc])
        else:
            S.act(lambda e, ps=ps: e.copy(out=utc[:, 4:8, :], in_=ps[:, :].rearrange("p (a b) -> p a b", b=128)), reads=["bank%d" % bi], writes=[nutc])
    S.dma(lambda e: e.dma_start(out=d["uts"][:, i, :], in_=utc[:].rearrange("p a b -> p (a b)")), reads=[nutc], writes=["uts%d" % (i // 2)], q="act")
    S.act(lambda e: e.copy(out=vbf[:], in_=vt[:]), reads=[nvt], writes=[nvbf])
    S.dma(lambda e: e.dma_start(out=d["vs"][:, i, :], in_=vbf[:]), reads=[nvbf], writes=["vs%d" % (i // 2)], q="act")


def norm_block(k, T2, src, rows0, acol, bcol, hT, hname, tag, src_deps=None, S=None, banks=(0, 1)):
    S = S or k.S
    for tt in range(2):
        xt = T2["xt"][tt]
        xn_ = T2["xtn"][tt] if "xtn" in T2 else "xt%s%d" % (tag, tt)
        S.dma(lambda e, xt=xt, tt=tt: e.dma_start(out=xt[:], in_=src[rows0 + tt * 128:rows0 + (tt + 1) * 128, :]), reads=([src_deps[tt]] if src_deps else []), writes=[xn_])
        junk = T2["junk"]
        ss = T2["ss"]
        xn = T2["xn"]
        S.act(lambda e, xt=xt, tt=tt: e.activation(out=junk[:], in_=xt[:], func=AF.Square, accum_out=ss[:, tt:tt + 1]), reads=[xn_], writes=[T2.get("junkn", "junk" + tag), "ss%s%d" % (tag, tt)])
        S.dve(lambda e, tt=tt: e.tensor_scalar(out=ss[:, tt:tt + 1], in0=ss[:, tt:tt + 1], scalar1=1.0 / D, scalar2=EPS, op0=ALU.mult, op1=ALU.add),
              reads=["ss%s%d" % (tag, tt)], writes=["ss%s%d" % (tag, tt)])
        S.pool(lambda e, tt=tt: e.tensor_tensor(out=ss[:, tt:tt + 1], in0=ss[:, tt:tt + 1], in1=k.neghalf[:, 0:1], op=ALU.pow),
               reads=["ss%s%d" % (tag, tt), "neghalf"], writes=["ss%s%d" % (tag, tt)])
        S.act(lambda e, xt=xt, tt=tt: e.activation(out=xn[:], in_=xt[:], func=AF.Copy, scale=ss[:, tt:tt + 1]), reads=[xn_, "ss%s%d" % (tag, tt)], writes=["xn" + tag])
        for half in range(2):
            ps = k.bank[banks[half]]
            for q in range(4):
                kc = half * 4 + q
                S.pe(lambda e, ps=ps, q=q, kc=kc: e.transpose(ps[:, q * 128:(q + 1) * 128], xn[:, kc * 128:(kc + 1) * 128], k.ident[:]),
                     reads=["xn" + tag, "ident"], writes=["bank%d" % banks[half]])
            tmp = T2["tmp"]
            S.dve(lambda e, ps=ps, half=half: e.tensor_tensor(out=tmp[:], in0=ps[:, :].rearrange("p (a b) -> p a b", b=128),
                                                              in1=acol[:, half * 4:(half + 1) * 4].unsqueeze(2).to_broadcast([128, 4, 128]), op=ALU.mult),
                  reads=["bank%d" % banks[half], "modcols"], writes=["ntmp" + tag])
            S.dve(lambda e, half=half, tt=tt: e.tensor_tensor(out=hT[:, half * 4:(half + 1) * 4, tt * 128:(tt + 1) * 128], in0=tmp[:],
                                                               in1=bcol[:, half * 4:(half + 1) * 4].unsqueeze(2).to_broadcast([128, 4, 128]), op=ALU.add),
                  reads=["ntmp" + tag, "modcols"], writes=[hname])


def load_cast(k, dst, dname, src_ap, stage, sname, eng="dve"):
    S = k.S
    S.dma(lambda e: e.dma_start(out=stage, in_=src_ap), writes=[sname])
    if eng == "dve":
        S.dve(lambda e: e.tensor_copy(out=dst, in_=stage), reads=[sname], writes=[dname])
    elif eng == "pool":
        S.pool(lambda e: e.tensor_copy(out=dst, in_=stage), reads=[sname], writes=[dname])
    else:
        S.act(lambda e: e.copy(out=dst, in_=stage), reads=[sname], writes=[dname])


def phase_1a(k):
    nc, S, d = k.nc, k.S, k.d
    with contextlib.ExitStack() as st:
        T = lambda n, s, dt=F32: k.T(n, s, dt, st)
        wqkv = T("wqkv", [128, 8, 1536], BF16)
        kaug = T("kaug", [128, 8, SEQ], BF16)
        vaug = T("vaug", [128, 32, 8, 65], BF16)
        qaug = [T("qaug0", [128, 8, BLK], BF16), T("qaug1", [128, 8, BLK], BF16)]
        qf = T("qf", [64, 8, BLK])
        kms = T("kms", [64, 8, 16])
        kmf = T("kmf", [64, 8, 16])
        xtA = T("xtA0", [128, D])
        xnA = T("xnA", [128, D])
        T2 = dict(xt=[xtA, xtA], junk=xnA, ss=T("ssA", [128, 2]), xn=xnA, tmp=T("ntmpA", [128, 4, 128]), xtn=["xtA0", "xtA0"], junkn="xnA")
        stage = [xtA[:, 0:768], xnA[:, 0:768]]
        hT = T("hTA", [128, 8, BLK], BF16)
        biasT = T("biasT", [128, 8, 16])
        causalneg = T("causalneg", [128, 2, BLK])
        maskb = T("maskb", [128, 16, 16])
        gm = T("gm", [128, 8, 16])
        mx8 = T("mx8", [128, 8, 8])
        Wall = T("Wall", [128, 8, 128])
        ssb = T("ssb", [128, 2, BLK])
        PT = [T("PT%d" % i, [128, 2, BLK], BF16) for i in range(3)]
        rinv = T("rinv", [128, 2])
        attn_tok = T("attn_tok", [128, 2, 512])

        for kc in range(8):
            for hf in range(2):
                load_cast(k, wqkv[:, kc, hf * 768:(hf + 1) * 768], "wqkv", d["w_in"][kc * 128:(kc + 1) * 128, hf * 768:(hf + 1) * 768], stage[hf], ("xtA0" if hf == 0 else "xnA"), eng=("dve" if hf == 0 else "pool"))
        S.dma(lambda e: e.dma_start(out=biasT[:], in_=d["biasT"]), writes=["biasT"])
        S.dma(lambda e: e.dma_start(out=causalneg[:], in_=d["causalneg"]), writes=["causalneg"])
        S.dma(lambda e: e.dma_start(out=maskb[:], in_=d["maskb"]), writes=["maskb"])
        for h in range(8):
            S.dma(lambda e, h=h: e.dma_start(out=kaug[64:82, h, :], in_=d["kconst"]), writes=["kaug_c"])
        for j in range(2):
            S.dma(lambda e, j=j: e.dma_start(out=qaug[j][80:82, :, :], in_=d["qal"]), writes=["qaug%d_c" % j])
        S.dve(lambda e: e.memset(vaug[:, :, :, 64:65], 1.0), writes=["vaug_c"])
        S.dve(lambda e: e.memset(Wall[:], 0.0), writes=["Wall"])
        S.dve(lambda e: e.memset(kms[:], 0.0), writes=["kms"])
        S.dve(lambda e: e.memset(kmf[:], 0.0), writes=["kmf"])
        S.pool(lambda e: e.memset(qaug[0][64:80, :, :], 0.0), writes=["qaug0_s"])

        acol = k.modcols[:, 0:8]
        bcol = k.modcols[:, 8:16]
        hTs = [hT, T("hTA1", [128, 8, BLK], BF16)]
        nb_run = k.opts.get("nb1a", NBLK)

        def block_prelude(S, b):
            qa = qaug[b % 2]
            qan = "qaug%d" % (b % 2)
            hTb = hTs[b % 2]
            hn = "hTA%d" % (b % 2)
            norm_block(k, T2, d["x"], b * BLK, acol, bcol, hTb, hn, "A", S=S)
            for h in range(8):
                qps = k.bank[2][0:64, 0:256]
                kps = k.bank[3][0:64, 0:256]
                for kc in range(8):
                    S.pe(lambda e, h=h, kc=kc, qps=qps: e.matmul(qps, lhsT=wqkv[:, kc, h * 64:(h + 1) * 64], rhs=hTb[:, kc, :], start=(kc == 0), stop=(kc == 7)),
                         reads=["wqkv", hn], writes=["bank2"])
                S.dve(lambda e, h=h, qps=qps: e.tensor_scalar(out=qf[:, h, :], in0=qps, scalar1=0.125, scalar2=None, op0=ALU.mult), reads=["bank2"], writes=["qf"])
                S.pool(lambda e, h=h, qa=qa: e.tensor_copy(out=qa[0:64, h, :], in_=qf[:, h, :]), reads=["qf"], writes=[qan + "_q"])
                for kc in range(8):
                    S.pe(lambda e, h=h, kc=kc, kps=kps: e.matmul(kps, lhsT=wqkv[:, kc, 512 + h * 64:512 + (h + 1) * 64], rhs=hTb[:, kc, :], start=(kc == 0), stop=(kc == 7)),
                         reads=["wqkv", hn], writes=["bank3"])
                S.act(lambda e, h=h, b=b, kps=kps: e.copy(out=kaug[0:64, h, b * BLK:(b + 1) * BLK], in_=kps), reads=["bank3"], writes=["kaug_%d" % b])
                S.dve(lambda e, h=h, b=b, kps=kps: e.tensor_reduce(out=kms[:, h, b:b + 1], in_=kps, axis=AX.X, op=ALU.add), reads=["bank3"], writes=["kms"])
            S.dve(lambda e, b=b: e.tensor_scalar(out=kmf[:, :, b:b + 1], in0=kms[:, :, b:b + 1], scalar1=1.0 / BLK, scalar2=None, op0=ALU.mult), reads=["kms"], writes=["kmf"])
            for tt in range(2):
                vps = k.bank[2 + tt]
                vpn = "bank%d" % (2 + tt)
                for kc in range(8):
                    S.pe(lambda e, tt=tt, kc=kc, vps=vps: e.matmul(vps[:, :], lhsT=hTb[:, kc, tt * 128:(tt + 1) * 128], rhs=wqkv[:, kc, 1024:1536], start=(kc == 0), stop=(kc == 7)),
                         reads=["wqkv", hn], writes=[vpn])
                S.act(lambda e, tt=tt, b=b, vps=vps: e.copy(out=vaug[:, 2 * b + tt, :, 0:64], in_=vps[:, :].rearrange("p (h c) -> p h c", c=64)), reads=[vpn], writes=["vaug_%d" % b])
            if b == 0:
                return
            for tt in range(2):
                gps = k.bank[0][:, 0:128].rearrange("p (h n) -> p h n", n=16)
                for h in range(8):
                    S.pe(lambda e, h=h, tt=tt, gps=gps: e.matmul(gps[:, h, :], lhsT=qf[:, h, tt * 128:(tt + 1) * 128], rhs=kmf[:, h, :], start=True, stop=True),
                         reads=["qf", "kmf"], writes=["bank0"])
                S.dve(lambda e, b=b, gps=gps: e.tensor_tensor(out=gm[:], in0=gps, in1=maskb[:, b, :].unsqueeze(1).to_broadcast([128, 8, 16]), op=ALU.add),
                      reads=["bank0", "maskb"], writes=["gm"])
                for h in range(8):
                    S.dve(lambda e, h=h: e.max(out=mx8[:, h, :], in_=gm[:, h, :]), reads=["gm"], writes=["mx8"])
                S.dve(lambda e: e.tensor_tensor(out=Wall[:, :, 64:80], in0=gm[:], in1=mx8[:, :, 2:3].to_broadcast([128, 8, 16]), op=ALU.is_ge),
                      reads=["gm", "mx8"], writes=["Wall"])
                S.dve(lambda e: e.tensor_scalar(out=Wall[:, :, 64:80], in0=Wall[:, :, 64:80], scalar1=-NEG, scalar2=NEG, op0=ALU.mult, op1=ALU.add),
                      reads=["Wall"], writes=["Wall"])
                S.dve(lambda e, b=b: e.memset(Wall[:, :, 64 + b:65 + b], 0.0), reads=["Wall"], writes=["Wall"])
                for hg in range(2):
                    wtp = k.bank[2 + hg]
                    wtn = "bank%d" % (2 + hg)
                    for j in range(4):
                        S.pe(lambda e, hg=hg, j=j, wtp=wtp: e.transpose(wtp[:, j * 128:(j + 1) * 128], Wall[:, hg * 4 + j, :], k.ident[:]), reads=["Wall", "ident"], writes=[wtn])
                    S.act(lambda e, hg=hg, tt=tt, qa=qa, wtp=wtp: e.copy(out=qa[64:80, hg * 4:(hg + 1) * 4, tt * 128:(tt + 1) * 128],
                                                                in_=wtp[64:80, :].rearrange("p (a b) -> p a b", b=128)), reads=[wtn], writes=[qan + "_s"])

        def block_attention(b, rec):
            qa = qaug[b % 2]
            qan = "qaug%d" % (b % 2)
            units = [(h, n) for h in range(8) for n in range(b + 1)]
            per = (len(rec.l) + len(units) - 1) // len(units)

            def unit_S(ui):
                h, n = units[ui]
                sb_i = 6 + (ui % 2)
                stp = k.bank[sb_i]
                for kh in range(2):
                    S.pe(lambda e, h=h, n=n, kh=kh, stp=stp: e.matmul(stp[:, kh * 256:(kh + 1) * 256], lhsT=kaug[0:82, h, (2 * n + kh) * 128:(2 * n + kh + 1) * 128],
                                                                     rhs=qa[0:82, h, :], start=True, stop=True),
                         reads=["kaug_c", "kaug_%d" % n, qan + "_q", qan + "_s", qan + "_c"], writes=["bank%d" % sb_i])

            def unit_EV(ui):
                h, n = units[ui]
                sb_i = 6 + (ui % 2)
                stp = k.bank[sb_i]
                stn = "bank%d" % sb_i
                pt = PT[ui % 3]
                ptn = "PT%d" % (ui % 3)
                acc = (k.bank[4][:, 0:130] if h % 2 == 0 else k.bank[5][:, 256:386]).rearrange("p (t c) -> p t c", c=65)
                accn = "bank%d" % (4 + h % 2)
                if n == b:
                    S.dve(lambda e, stp=stp: e.tensor_tensor(out=ssb[:].rearrange("p a b -> p (a b)"), in0=stp[:, :], in1=causalneg[:].rearrange("p a b -> p (a b)"), op=ALU.add),
                          reads=[stn, "causalneg"], writes=["ssb"])
                    S.act(lambda e, pt=pt, h=h: e.activation(out=pt[:].rearrange("p a b -> p (a b)"), in_=ssb[:].rearrange("p a b -> p (a b)"), func=AF.Exp, bias=biasT[:, h, 0:1]),
                          reads=["ssb", "biasT"], writes=[ptn])
                else:
                    S.act(lambda e, pt=pt, h=h, stp=stp, df=b - n: e.activation(out=pt[:].rearrange("p a b -> p (a b)"), in_=stp[:, :], func=AF.Exp, bias=biasT[:, h, df:df + 1]),
                          reads=[stn, "biasT"], writes=[ptn])
                for kh in range(2):
                    for qt in range(2):
                        S.pe(lambda e, pt=pt, kh=kh, qt=qt, n=n, h=h, acc=acc: e.matmul(acc[:, qt, :], lhsT=pt[:, kh, qt * 128:(qt + 1) * 128], rhs=vaug[:, 2 * n + kh, h, :],
                                                                                      start=(n == 0 and kh == 0 and qt == 0), stop=(n == b and kh == 1), skip_group_check=True),
                             reads=[ptn, "vaug_%d" % n, "vaug_c"], writes=[accn])
                if n == b:
                    S.dve(lambda e, acc=acc: e.reciprocal(out=rinv[:], in_=acc[:, :, 64]), reads=[accn], writes=["rinv"])
                    S.dve(lambda e, acc=acc, h=h: e.tensor_tensor(out=attn_tok[:, :, h * 64:(h + 1) * 64], in0=acc[:, :, 0:64], in1=rinv[:].unsqueeze(2).to_broadcast([128, 2, 64]), op=ALU.mult),
                          reads=[accn, "rinv"], writes=["attn_tok"])

            unit_S(0)
            for ui in range(len(units)):
                if ui + 1 < len(units):
                    unit_S(ui + 1)
                unit_EV(ui)
                rec.replay(S, per)
            rec.replay(S)
            for qt in range(2):
                tp = k.bank[2 + qt]
                tpn = "bank%d" % (2 + qt)
                for ac in range(4):
                    S.pe(lambda e, qt=qt, ac=ac, tp=tp: e.transpose(tp[:, ac * 128:(ac + 1) * 128], attn_tok[:, qt, ac * 128:(ac + 1) * 128], k.ident[:]), reads=["attn_tok", "ident"], writes=[tpn])
                S.act(lambda e, qt=qt, b=b, tp=tp: e.copy(out=k.attnT[:, :, b * BLK + qt * 128:b * BLK + (qt + 1) * 128], in_=tp[:, :].rearrange("p (a b) -> p a b", b=128)),
                      reads=[tpn], writes=["attnT_%d" % b])

        block_prelude(S, 0)
        for b in range(nb_run):
            rec = Rec()
            if b + 1 < nb_run:
                block_prelude(rec, b + 1)
            block_attention(b, rec)

        if "1a" in k.dbg:
            o = k.dbg_tensor("attnT", [128, 4, SEQ], BF16)
            nb_ = k.opts.get("nb1a", NBLK)
            S.dma(lambda e: e.dma_start(out=o, in_=k.attnT[:]), reads=["attnT_%d" % b for b in range(nb_)])
            o2 = k.dbg_tensor("kaug", [128, 8, SEQ], BF16)
            S.dma(lambda e: e.dma_start(out=o2, in_=kaug[:]), reads=["kaug_c"] + ["kaug_%d" % b for b in range(nb_)])
            o3 = k.dbg_tensor("qaug1", [128, 8, BLK], BF16)
            qi_ = (nb_ - 1) % 2
            S.dma(lambda e: e.dma_start(out=o3, in_=qaug[qi_][:]), reads=["qaug%d_q" % qi_, "qaug%d_s" % qi_, "qaug%d_c" % qi_])
            o5 = k.dbg_tensor("attn_tok", [128, 2, 512])
            S.dma(lambda e: e.dma_start(out=o5, in_=attn_tok[:]), reads=["attn_tok"])
            o6 = k.dbg_tensor("PT0", [128, 2, BLK], BF16)
            S.dma(lambda e: e.dma_start(out=o6, in_=PT[0][:]), reads=["PT0"])
            o7 = k.dbg_tensor("ssb", [128, 2, BLK])
            S.dma(lambda e: e.dma_start(out=o7, in_=ssb[:]), reads=["ssb"])
            o8 = k.dbg_tensor("rinv", [128, 2])
            S.dma(lambda e: e.dma_start(out=o8, in_=rinv[:]), reads=["rinv"])
            o4 = k.dbg_tensor("vaug", [128, 32, 8, 65], BF16)
            S.dma(lambda e: e.dma_start(out=o4, in_=vaug[:]), reads=["vaug_c"] + ["vaug_%d" % b for b in range(nb_)])


def phase_1b(k):
    nc, S, d = k.nc, k.S, k.d
    with contextlib.ExitStack() as st:
        T = lambda n, s, dt=F32: k.T(n, s, dt, st)
        wpg = T("wpg", [128, 8, 2560], BF16)
        stage = [T("wstb0", [128, 1280]), T("wstb1", [128, 1280])]
        wau = T("wau", [128, 4, D], BF16)
        wpu = T("wpu", [128, 4, D], BF16)
        wout = T("wout", [128, 8, D], BF16)
        wmix = T("wmix", [128, 4, 128], BF16)
        bmix = T("bmixc", [128, 4])
        pscale = T("pscalec", [128, 4])
        invc = T("invc", [128, 4, 16])
        T2 = dict(xt=[T("xtB0", [128, D]), T("xtB1", [128, D])], junk=T("junkB", [128, D], BF16), ss=T("ssB", [128, 2]), xn=T("xnB", [128, D]), tmp=T("ntmpB", [128, 4, 128]))
        hT = T("hTB", [128, 8, BLK], BF16)
        pbuf = [T("pbuf0", [128, 4, 272]), T("pbuf1", [128, 4, 272])]
        s2 = T("s2", [128, 4, 272])
        s4 = T("s4", [128, 4, 272])
        s8 = T("s8", [128, 4, 272])
        s16 = T("s16", [128, 4, 272])
        t16 = T("t16", [128, 16])
        pin = T("pin", [128, 4, BLK], BF16)
        poolT = T("poolT", [128, 4, BLK], BF16)
        sga = [T("sga0", [128, BLK]), T("sga1", [128, BLK])]
        sgb = [T("sgb0", [128, BLK]), T("sgb1", [128, BLK])]
        m1 = T("m1", [128, BLK])
        m2 = T("m2", [128, BLK])
        mergedT = T("mergedT", [128, 8, BLK], BF16)
        x1t = [T("x1t0", [128, D]), T("x1t1", [128, D])]
        u_ut = [T("u_ut0", [128, D]), T("u_ut1", [128, D])]
        u_vt = [T("u_vt0", [128, D]), T("u_vt1", [128, D])]
        u_utc = T("u_utc", [128, 8, 128], BF16)
        u_vbf = T("u_vbf", [128, D], BF16)
        u_next = [0]
        gt1b = T("gt1b", [128, D])
        S.dma(lambda e: e.dma_start(out=gt1b[:], in_=d["bcs"][0]), reads=["bcs0"], writes=["gt1b"])

        for kc in range(8):
            for hf in range(2):
                load_cast(k, wpg[:, kc, hf * 1280:(hf + 1) * 1280], "wpg", d["w_in"][kc * 128:(kc + 1) * 128, 1536 + hf * 1280:1536 + (hf + 1) * 1280], stage[hf][:], "wstb%d" % hf, eng=("dve" if hf == 0 else "pool"))
        for kc in range(4):
            load_cast(k, wau[:, kc, :], "wau", d["w_au"][kc * 128:(kc + 1) * 128, :], stage[kc % 2][:, 0:D], "wstb%d" % (kc % 2), eng="pool")
        for kc in range(4):
            load_cast(k, wpu[:, kc, :], "wpu", d["w_pu"][kc * 128:(kc + 1) * 128, :], stage[kc % 2][:, 0:D], "wstb%d" % (kc % 2), eng="dve")
        for kc in range(8):
            load_cast(k, wout[:, kc, :], "wout", d["w_out"][kc * 128:(kc + 1) * 128, :], stage[kc % 2][:, 0:D], "wstb%d" % (kc % 2), eng=("dve" if kc % 2 == 0 else "pool"))
        for g in range(4):
            load_cast(k, wmix[:, g, :], "wmix", d["w_mix"][g], stage[g % 2][:, 0:128], "wstb%d" % (g % 2), eng="dve")
        S.dma(lambda e: e.dma_start(out=bmix[:], in_=d["b_mix"]), writes=["bmixc"])
        S.dma(lambda e: e.dma_start(out=pscale[:], in_=d["pscale"]), writes=["pscalec"])
        S.dma(lambda e: e.dma_start(out=invc[:], in_=d["invcnt16"]), writes=["invc"])
        S.dve(lambda e: e.memset(pbuf[0][:, :, 0:16], 0.0), writes=["pbuf0"])

        acol = k.modcols[:, 0:8]
        bcol = k.modcols[:, 8:16]
        winsrc = [s2, s4, s8, s16]
        winname = ["s2", "s4", "s8", "s16"]
        for b in range(k.opts.get("nb1b", NBLK)):
            pb = pbuf[b % 2]
            pbn = "pbuf%d" % (b % 2)
            norm_block(k, T2, d["x"], b * BLK, acol, bcol, hT, "hTB", "B")
            if b > 0:
                pprev = pbuf[(b - 1) % 2]
                S.dve(lambda e, pb=pb, pprev=pprev: e.tensor_copy(out=pb[:, :, 0:16], in_=pprev[:, :, 256:272]), reads=["pbuf%d" % ((b - 1) % 2)], writes=[pbn])
            for g in range(4):
                pps = k.bank[2 + g % 2][:, 0:256]
                ppn = "bank%d" % (2 + g % 2)
                for kc in range(8):
                    S.pe(lambda e, g=g, kc=kc, pps=pps: e.matmul(pps, lhsT=wpg[:, kc, g * 128:(g + 1) * 128], rhs=hT[:, kc, :], start=(kc == 0), stop=(kc == 7)),
                         reads=["wpg", "hTB"], writes=[ppn])
                S.act(lambda e, g=g, pb=pb, pps=pps: e.copy(out=pb[:, g, 16:272], in_=pps), reads=[ppn], writes=[pbn])
            S.dve(lambda e, pb=pb: e.tensor_tensor(out=s2[:, :, 1:272], in0=pb[:, :, 1:272], in1=pb[:, :, 0:271], op=ALU.add), reads=[pbn], writes=["s2"])
            S.pool(lambda e: e.tensor_tensor(out=s4[:, 1:4, 3:272], in0=s2[:, 1:4, 3:272], in1=s2[:, 1:4, 1:270], op=ALU.add), reads=["s2"], writes=["s4"])
            S.dve(lambda e: e.tensor_tensor(out=s8[:, 2:4, 7:272], in0=s4[:, 2:4, 7:272], in1=s4[:, 2:4, 3:268], op=ALU.add), reads=["s4"], writes=["s8"])
            S.pool(lambda e: e.tensor_tensor(out=s16[:, 3:4, 15:272], in0=s8[:, 3:4, 15:272], in1=s8[:, 3:4, 7:264], op=ALU.add), reads=["s8"], writes=["s16"])
            for g in range(4):
                w = float(2 ** (g + 1))
                S.dve(lambda e, g=g, w=w, pb=pb: e.scalar_tensor_tensor(out=pin[:, g, :], in0=winsrc[g][:, g, 16:272], scalar=1.0 / w, in1=pb[:, g, 16:272], op0=ALU.mult, op1=ALU.subtract),
                      reads=[winname[g], pbn], writes=["pin"])
                if b == 0:
                    S.dve(lambda e, g=g: e.tensor_tensor(out=t16[:], in0=winsrc[g][:, g, 16:32], in1=invc[:, g, :], op=ALU.mult), reads=[winname[g], "invc"], writes=["t16"])
                    S.dve(lambda e, g=g, pb=pb: e.tensor_tensor(out=pin[:, g, 0:16], in0=t16[:], in1=pb[:, g, 16:32], op=ALU.subtract), reads=["t16", pbn], writes=["pin"])
            for g in range(4):
                mps = k.bank[2 + g % 2][:, 0:256]
                mpn = "bank%d" % (2 + g % 2)
                S.pe(lambda e, g=g, mps=mps: e.matmul(mps, lhsT=wmix[:, g, :], rhs=pin[:, g, :], start=True, stop=True), reads=["wmix", "pin"], writes=[mpn])
                S.dve(lambda e, g=g, mps=mps: e.tensor_scalar(out=poolT[:, g, :], in0=mps, scalar1=bmix[:, g:g + 1], scalar2=pscale[:, g:g + 1], op0=ALU.add, op1=ALU.mult),
                      reads=[mpn, "bmixc", "pscalec"], writes=["poolT"])
            for dc in range(8):
                st_ = dc % 2
                bA = k.bank[4 + 2 * st_]
                bB = k.bank[5 + 2 * st_]
                nA = "bank%d" % (4 + 2 * st_)
                nB = "bank%d" % (5 + 2 * st_)
                aups, pups = bA[:, 0:256], bA[:, 256:512]
                gaps, gbps = bB[:, 0:256], bB[:, 256:512]
                for kc in range(4):
                    S.pe(lambda e, dc=dc, kc=kc, aups=aups, b=b: e.matmul(aups, lhsT=wau[:, kc, dc * 128:(dc + 1) * 128], rhs=k.attnT[:, kc, b * BLK:(b + 1) * BLK], start=(kc == 0), stop=(kc == 3)),
                         reads=["wau", "attnT_%d" % b], writes=[nA])
                for kc in range(4):
                    S.pe(lambda e, dc=dc, kc=kc, pups=pups: e.matmul(pups, lhsT=wpu[:, kc, dc * 128:(dc + 1) * 128], rhs=poolT[:, kc, :], start=(kc == 0), stop=(kc == 3)),
                         reads=["wpu", "poolT"], writes=[nA])
                for kc in range(8):
                    S.pe(lambda e, dc=dc, kc=kc, gaps=gaps: e.matmul(gaps, lhsT=wpg[:, kc, 512 + dc * 128:512 + (dc + 1) * 128], rhs=hT[:, kc, :], start=(kc == 0), stop=(kc == 7)),
                         reads=["wpg", "hTB"], writes=[nB])
                for kc in range(8):
                    S.pe(lambda e, dc=dc, kc=kc, gbps=gbps: e.matmul(gbps, lhsT=wpg[:, kc, 1536 + dc * 128:1536 + (dc + 1) * 128], rhs=hT[:, kc, :], start=(kc == 0), stop=(kc == 7)),
                         reads=["wpg", "hTB"], writes=[nB])
                S.act(lambda e, st_=st_, gaps=gaps: e.activation(out=sga[st_][:], in_=gaps, func=AF.Sigmoid), reads=[nB], writes=["sga%d" % st_])
                S.act(lambda e, st_=st_, gbps=gbps: e.activation(out=sgb[st_][:], in_=gbps, func=AF.Sigmoid), reads=[nB], writes=["sgb%d" % st_])
                S.dve(lambda e, st_=st_, aups=aups: e.tensor_tensor(out=m1[:], in0=sga[st_][:], in1=aups, op=ALU.mult), reads=["sga%d" % st_, nA], writes=["m1"])
                S.dve(lambda e, st_=st_, pups=pups: e.tensor_tensor(out=m2[:], in0=sgb[st_][:], in1=pups, op=ALU.mult), reads=["sgb%d" % st_, nA], writes=["m2"])
                S.pool(lambda e, dc=dc: e.tensor_tensor(out=mergedT[:, dc, :], in0=m1[:], in1=m2[:], op=ALU.add), reads=["m1", "m2"], writes=["mergedT"])
                if k.opts.get("u_in_1b", True) and u_next[0] < 128:
                    ui_ = u_next[0]
                    if ui_ == 0:
                        u_load(k, S, 0, u_ut[0], u_vt[0], ("u_ut0", "u_vt0"))
                    if ui_ + 1 < 128:
                        un_ = (ui_ + 1) % 2
                        u_load(k, S, ui_ + 1, u_ut[un_], u_vt[un_], ("u_ut%d" % un_, "u_vt%d" % un_))
                    up_ = ui_ % 2
                    u_proc(k, S, ui_, u_ut[up_], u_vt[up_], u_utc, u_vbf, (0, 1), ("u_ut%d" % up_, "u_vt%d" % up_, "u_utc", "u_vbf"))
                    u_next[0] += 1
            for tt in range(2):
                xo = x1t[tt]
                xon = "x1t%d" % tt
                for dh in range(2):
                    yps = k.bank[2 + dh]
                    ypn = "bank%d" % (2 + dh)
                    for kc in range(8):
                        S.pe(lambda e, tt=tt, dh=dh, kc=kc, yps=yps: e.matmul(yps[:, :], lhsT=mergedT[:, kc, tt * 128:(tt + 1) * 128], rhs=wout[:, kc, dh * 512:(dh + 1) * 512], start=(kc == 0), stop=(kc == 7)),
                             reads=["mergedT", "wout"], writes=[ypn])
                    S.dve(lambda e, dh=dh, xo=xo, yps=yps: e.tensor_tensor(out=xo[:, dh * 512:(dh + 1) * 512], in0=yps[:, :], in1=gt1b[:, dh * 512:(dh + 1) * 512], op=ALU.mult),
                          reads=[ypn, "gt1b"], writes=[xon])
                S.pool(lambda e, xo=xo, tt=tt: e.tensor_tensor(out=xo[:], in0=xo[:], in1=T2["xt"][tt][:], op=ALU.add), reads=[xon, "xtB%d" % tt], writes=[xon])
                r0 = b * BLK + tt * 128
                S.dma(lambda e, xo=xo, r0=r0: e.dma_start(out=d["x1s"][r0:r0 + 128, :], in_=xo[:]), reads=[xon], writes=["x1s_%d_%d" % (b, tt)])
                if "1b" in k.dbg:
                    if "x1" not in k.dbg_out:
                        k.dbg_tensor("x1", [SEQ, D])
                    ox = k.dbg_out["x1"]
                    S.dma(lambda e, xo=xo, r0=r0, ox=ox: e.dma_start(out=ox[r0:r0 + 128, :], in_=xo[:]), reads=[xon])
        if "1b" in k.dbg:
            pass


def phase_2(k):
    nc, S, d = k.nc, k.S, k.d
    with contextlib.ExitStack() as st:
        T = lambda n, s, dt=F32: k.T(n, s, dt, st)
        wq = T("wq", [128, 8, 2048], BF16)
        gt2b = T("gt2b", [128, D])
        gfb = T("gfb", [128, D])
        S.dma(lambda e: e.dma_start(out=gt2b[:], in_=d["bcs"][1]), reads=["bcs1"], writes=["gt2b"])
        S.dma(lambda e: e.dma_start(out=gfb[:], in_=d["bcs"][2]), reads=["bcs2"], writes=["gfb"])
        keysT = T("keysT", [128, 2, 128])
        kst = T("kst", [128, 128])
        iota128 = T("iota128", [128, 128])
        iota16 = T("iota16", [128, 16])
        T2 = dict(xt=[T("xtC0", [128, D]), T("xtC1", [128, D])], junk=T("junkC", [128, D], BF16), ss=T("ssC", [128, 2]), xn=T("xnC", [128, D]), tmp=T("ntmpC", [128, 4, 128]))
        stage = [T2["xt"][0], T2["xt"][1]]
        h2Ts = [T("h2T0", [128, 8, BLK], BF16), T("h2T1", [128, 8, BLK], BF16)]
        TS = 4
        NR = 8
        Rbuf = T("Rbuf", [128, 2 * NR, TS * 128], BF16)
        Rn = ["R1_%d" % i for i in range(NR)] + ["R2_%d" % i for i in range(NR)]
        R1 = [Rbuf[:, i, :].rearrange("p (t c) -> p t c", c=128) for i in range(NR)]
        R2 = [Rbuf[:, NR + i, :].rearrange("p (t c) -> p t c", c=128) for i in range(NR)]
        qpT = Rbuf[:].rearrange("p a b -> p (a b)").bitcast(F32).rearrange("p (m t) -> p m t", t=BLK)
        s_sb = T("s_sb", [128, 16, 128])
        s_sb2 = T("s_sb2", [128, 16, 128])
        vals = T("vals", [128, 16, 16])
        idxu = T("idxu", [128, 16, 16], U32)
        idxf = T("idxf", [128, 16, 16])
        cand = s_sb[:].rearrange("p m n -> p (m n)").rearrange("p (h c) -> p h c", c=256)
        cand2 = s_sb2[:].rearrange("p m n -> p (m n)").rearrange("p (h c) -> p h c", c=256)
        tops = T("tops", [128, 8, 16])
        flatu = T("flatu", [128, 8, 16], U32)
        aidxu = T("aidxu", [128, 8, 16], U32)
        bidxu = T("bidxu", [128, 8, 16], U32)
        aidx = T("aidx", [128, 8, 16])
        bidx = T("bidx", [128, 8, 16])
        gate = T("gate", [128, 8, 16])
        gsum = T("gsum", [128, 8])
        e1f = T("e1f", [128, 128])
        e2f = T("e2f", [128, 128])
        e1T = T("e1T", [128, BLK])
        e2T = T("e2T", [128, BLK])
        gateT = T("gateT", [128, BLK])
        GT = T("GT", [128, BLK, 128], BF16)
        NI = 2
        NSB = 3
        utb = [T("utb%d" % i, [128, NI, 8, 128], BF16) for i in range(NSB)]
        vb = [T("vb%d" % i, [128, NI, D], BF16) for i in range(NSB)]
        gl = [T("gl%d" % i, [128, BLK], BF16) for i in range(2)]
        actT = [T("actT%d" % i, [128, BLK], BF16) for i in range(2)]
        x2 = T("x2", [128, D])
        ss2 = T("ss2", [128, 1])
        ot = [s_sb[:].rearrange("p m n -> p (m n)")[:, 0:D], s_sb2[:].rearrange("p m n -> p (m n)")[:, 0:D]]
        otn = ["s_sb", "s_sb2"]

        for kc in range(8):
            for hf in range(2):
                load_cast(k, wq[:, kc, hf * 1024:(hf + 1) * 1024], "wq", d["w_pq"][kc * 128:(kc + 1) * 128, hf * 1024:(hf + 1) * 1024], stage[hf][:], "xtC%d" % hf, eng=("dve" if hf == 0 else "pool"))
        S.dma(lambda e: e.dma_start(out=iota128[:], in_=d["iota128"]), writes=["iota128"])
        S.dma(lambda e: e.dma_start(out=iota16[:], in_=d["iota16"]), writes=["iota16"])
        iota_bf = T("iota_bf", [128, 128], BF16)
        S.dve(lambda e: e.tensor_copy(out=iota_bf[:], in_=iota128[:]), reads=["iota128"], writes=["iota_bf"])
        for p in range(2):
            S.dma(lambda e, p=p: e.dma_start(out=kst[:], in_=d["keys"][p]), writes=["kst"])
            S.pe(lambda e: e.transpose(k.bank[7][:, 0:128], kst[:], k.ident[:]), reads=["kst", "ident"], writes=["bank7"])
            S.dve(lambda e, p=p: e.tensor_copy(out=keysT[:, p, :], in_=k.bank[7][:, 0:128]), reads=["bank7"], writes=["keysT"])

        acol = k.modcols[:, 16:24]
        bcol = k.modcols[:, 24:32]

        def prelude(S, g):
            h2T = h2Ts[g % 2]
            hn = "h2T%d" % (g % 2)
            norm_block(k, T2, d["x1s"], g * BLK, acol, bcol, h2T, hn, "C", src_deps=["x1s_%d_%d" % (g, tt_) for tt_ in range(2)], S=S, banks=(6, 7))
            for m in range(16):
                bi = 6 + m % 2
                qps = k.bank[bi][:, 0:256]
                qpn = "bank%d" % bi
                for kc in range(8):
                    S.pe(lambda e, m=m, kc=kc, qps=qps: e.matmul(qps, lhsT=wq[:, kc, m * 128:(m + 1) * 128], rhs=h2T[:, kc, :], start=(kc == 0), stop=(kc == 7)),
                         reads=["wq", hn], writes=[qpn])
                if m % 2 == 0:
                    S.act(lambda e, m=m, qps=qps: e.copy(out=qpT[:, m, :], in_=qps), reads=[qpn], writes=Rn)
                else:
                    S.dve(lambda e, m=m, qps=qps: e.tensor_copy(out=qpT[:, m, :], in_=qps), reads=[qpn], writes=Rn)
            for tt in range(2):
                for mg in range(4):
                    sps = k.bank[7]
                    for mm in range(4):
                        m = mg * 4 + mm
                        S.pe(lambda e, m=m, mm=mm, tt=tt, sps=sps: e.matmul(sps[:, mm * 128:(mm + 1) * 128], lhsT=qpT[:, m, tt * 128:(tt + 1) * 128], rhs=keysT[:, m % 2, :], start=True, stop=True),
                             reads=Rn + ["keysT"], writes=["bank7"])
                    S.act(lambda e, mg=mg, sps=sps: e.copy(out=s_sb[:, mg * 4:(mg + 1) * 4, :], in_=sps[:, :].rearrange("p (a b) -> p a b", b=128)), reads=["bank7"], writes=["s_sb"])
                for m in range(16):
                    S.dve(lambda e, m=m: e.max(out=vals[:, m, 0:8], in_=s_sb[:, m, :]), reads=["s_sb"], writes=["vals"])
                    S.dve(lambda e, m=m: e.max_index(out=idxu[:, m, 0:8], in_max=vals[:, m, 0:8], in_values=s_sb[:, m, :]), reads=["s_sb", "vals"], writes=["idxu"])
                    S.dve(lambda e, m=m: e.match_replace(out=s_sb2[:, m, :], in_to_replace=vals[:, m, 0:8], in_values=s_sb[:, m, :], imm_value=-1e30), reads=["s_sb", "vals"], writes=["s_sb2"])
                    S.dve(lambda e, m=m: e.max(out=vals[:, m, 8:16], in_=s_sb2[:, m, :]), reads=["s_sb2"], writes=["vals"])
                    S.dve(lambda e, m=m: e.max_index(out=idxu[:, m, 8:16], in_max=vals[:, m, 8:16], in_values=s_sb2[:, m, :]), reads=["s_sb2", "vals"], writes=["idxu"])
                S.pool(lambda e: e.tensor_copy(out=idxf[:], in_=idxu[:]), reads=["idxu"], writes=["idxf"])
                v4 = vals[:].rearrange("p (h two) k -> p h two k", two=2)
                S.dve(lambda e, v4=v4: e.tensor_tensor(out=cand[:].rearrange("p h (a b) -> p h a b", b=16), in0=v4[:, :, 0, :].unsqueeze(3).to_broadcast([128, 8, 16, 16]),
                                                      in1=v4[:, :, 1, :].unsqueeze(2).to_broadcast([128, 8, 16, 16]), op=ALU.add), reads=["vals"], writes=["s_sb"])
                for h in range(8):
                    S.dve(lambda e, h=h: e.max(out=tops[:, h, 0:8], in_=cand[:, h, :]), reads=["s_sb"], writes=["tops"])
                    S.dve(lambda e, h=h: e.max_index(out=flatu[:, h, 0:8], in_max=tops[:, h, 0:8], in_values=cand[:, h, :]), reads=["s_sb", "tops"], writes=["flatu"])
                    S.dve(lambda e, h=h: e.match_replace(out=cand2[:, h, :], in_to_replace=tops[:, h, 0:8], in_values=cand[:, h, :], imm_value=-1e30), reads=["s_sb", "tops"], writes=["s_sb2"])
                    S.dve(lambda e, h=h: e.max(out=tops[:, h, 8:16], in_=cand2[:, h, :]), reads=["s_sb2"], writes=["tops"])
                    S.dve(lambda e, h=h: e.max_index(out=flatu[:, h, 8:16], in_max=tops[:, h, 8:16], in_values=cand2[:, h, :]), reads=["s_sb2", "tops"], writes=["flatu"])
                S.dve(lambda e: e.tensor_tensor(out=gate[:], in0=tops[:], in1=tops[:, :, 0:1].to_broadcast([128, 8, 16]), op=ALU.subtract), reads=["tops"], writes=["gate"])
                S.act(lambda e: e.activation(out=gate[:], in_=gate[:], func=AF.Exp), reads=["gate"], writes=["gate"])
                S.dve(lambda e: e.tensor_reduce(out=gsum[:], in_=gate[:], axis=AX.X, op=ALU.add), reads=["gate"], writes=["gsum"])
                S.dve(lambda e: e.reciprocal(out=gsum[:], in_=gsum[:]), reads=["gsum"], writes=["gsum"])
                S.dve(lambda e: e.tensor_tensor(out=gate[:], in0=gate[:], in1=gsum[:].unsqueeze(2).to_broadcast([128, 8, 16]), op=ALU.mult), reads=["gate", "gsum"], writes=["gate"])
                S.dve(lambda e: e.tensor_scalar(out=aidxu[:], in0=flatu[:], scalar1=4, scalar2=None, op0=ALU.logical_shift_right), reads=["flatu"], writes=["aidxu"])
                S.dve(lambda e: e.tensor_scalar(out=bidxu[:], in0=flatu[:], scalar1=15, scalar2=None, op0=ALU.bitwise_and), reads=["flatu"], writes=["bidxu"])
                S.pool(lambda e: e.tensor_copy(out=aidx[:], in_=aidxu[:]), reads=["aidxu"], writes=["aidx"])
                S.pool(lambda e: e.tensor_copy(out=bidx[:], in_=bidxu[:]), reads=["bidxu"], writes=["bidx"])
                i4 = idxf[:].rearrange("p (h two) k -> p h two k", two=2)
                eq = cand[:].rearrange("p h (a b) -> p (h a) b", b=16)
                eq4 = cand[:].rearrange("p h (a b) -> p h a b", b=16)
                for which, (src_idx, dst) in enumerate(((aidx, e1f), (bidx, e2f))):
                    S.dve(lambda e, src_idx=src_idx: e.tensor_tensor(out=eq, in0=src_idx[:].rearrange("p h k -> p (h k)").unsqueeze(2).to_broadcast([128, 128, 16]),
                                                                     in1=iota16[:].unsqueeze(1).to_broadcast([128, 128, 16]), op=ALU.is_equal), reads=["aidx", "bidx", "iota16"], writes=["s_sb"])
                    S.dve(lambda e, which=which: e.tensor_tensor(out=eq4, in0=eq4, in1=i4[:, :, which, :].unsqueeze(2).to_broadcast([128, 8, 16, 16]), op=ALU.mult), reads=["s_sb", "idxf"], writes=["s_sb"])
                    S.dve(lambda e, dst=dst: e.tensor_reduce(out=dst[:], in_=eq, axis=AX.X, op=ALU.add), reads=["s_sb"], writes=["e1f" if which == 0 else "e2f"])
                for (src, sn, dst, dn, col) in ((e1f, "e1f", e1T, "e1T", 0), (e2f, "e2f", e2T, "e2T", 1), (gate, "gate", gateT, "gateT", 2)):
                    src_ap = src[:] if src is not gate else gate[:].rearrange("p h k -> p (h k)")
                    S.pe(lambda e, src_ap=src_ap, col=col: e.transpose(k.bank[6][:, col * 128:(col + 1) * 128], src_ap, k.ident[:]), reads=[sn, "ident"], writes=["bank6"])
                    S.act(lambda e, dst=dst, col=col, tt=tt: e.copy(out=dst[:, tt * 128:(tt + 1) * 128], in_=k.bank[6][:, col * 128:(col + 1) * 128]), reads=["bank6"], writes=[dn])

        def gcon(S, g):
            for sc in range(BLK // TS):
                r1 = R1[sc % NR]; r2 = R2[sc % NR]
                n1 = "R1_%d" % (sc % NR); n2 = "R2_%d" % (sc % NR)
                for tl in range(TS):
                    tcol = sc * TS + tl
                    S.dve(lambda e, r1=r1, tl=tl, tcol=tcol: e.tensor_scalar(out=r1[:, tl, :], in0=iota_bf[:], scalar1=e1T[:, tcol:tcol + 1], scalar2=None, op0=ALU.is_equal),
                                                        reads=["iota_bf", "e1T"], writes=[n1])
                    S.dve(lambda e, r2=r2, tl=tl, tcol=tcol: e.tensor_scalar(out=r2[:, tl, :], in0=iota_bf[:], scalar1=e2T[:, tcol:tcol + 1], scalar2=gateT[:, tcol:tcol + 1], op0=ALU.is_equal, op1=ALU.mult),
                          reads=["iota_bf", "e2T", "gateT"], writes=[n2])
                for q4 in range(TS // 4):
                    gb = 5 if (q4 % 2 == 0) else 4
                    gps = k.bank[gb]
                    for tl in range(4):
                        t_in = q4 * 4 + tl
                        S.pe(lambda e, gps=gps, tl=tl, t_in=t_in, r1=r1, r2=r2: e.matmul(gps[:, tl * 128:(tl + 1) * 128], lhsT=r2[:, t_in, :], rhs=r1[:, t_in, :], start=True, stop=True),
                             reads=[n1, n2], writes=["bank%d" % gb])
                    t0 = sc * TS + q4 * 4
                    S.act(lambda e, gps=gps, t0=t0: e.copy(out=GT[:, t0:t0 + 4, :], in_=gps[:, :].rearrange("p (a b) -> p a b", b=128)), reads=["bank%d" % gb], writes=["GT"])

        def main_load(S, g, ip):
            sb = ip % NSB
            S.dma(lambda e, ip=ip, sb=sb: e.dma_start(out=utb[sb][:].rearrange("p n a b -> p (n a b)"), in_=d["uts"][:, NI * ip:NI * (ip + 1), :].rearrange("p n c -> p (n c)")),
                  reads=["uts%d" % ip], writes=["utb%d" % sb])
            S.dma(lambda e, ip=ip, sb=sb: e.dma_start(out=vb[sb][:].rearrange("p n c -> p (n c)"), in_=d["vs"][:, NI * ip:NI * (ip + 1), :].rearrange("p n c -> p (n c)")),
                  reads=["vs%d" % ip], writes=["vb%d" % sb])

        def main_A(S, g, i):
            h2T = h2Ts[g % 2]
            hn = "h2T%d" % (g % 2)
            sb = (i // NI) % NSB
            ii = i % NI
            bi = 4 + i % 2
            aps = k.bank[bi][:, 0:256]
            for kc in range(8):
                S.pe(lambda e, sb=sb, ii=ii, kc=kc, aps=aps: e.matmul(aps, lhsT=utb[sb][:, ii, kc, :], rhs=h2T[:, kc, :], start=(kc == 0), stop=(kc == 7)), reads=["utb%d" % sb, hn], writes=["bank%d" % bi])

        def main_EV(S, g, i):
            sb = (i // NI) % NSB
            ii = i % NI
            bi = 4 + i % 2
            aps = k.bank[bi][:, 0:256]
            S.act(lambda e, i=i, aps=aps: e.activation(out=gl[i % 2][:], in_=aps, func=AF.Gelu_apprx_tanh), reads=["bank%d" % bi], writes=["gl%d" % (i % 2)])
            S.pool(lambda e, i=i: e.tensor_tensor(out=actT[i % 2][:], in0=gl[i % 2][:], in1=GT[:, :, i], op=ALU.mult), reads=["gl%d" % (i % 2), "GT"], writes=["actT%d" % (i % 2)])
            for tt in range(2):
                for dh in range(2):
                    yb = tt * 2 + dh
                    S.pe(lambda e, i=i, ii=ii, tt=tt, dh=dh, yb=yb, sb=sb: e.matmul(k.bank[yb][:, :], lhsT=actT[i % 2][:, tt * 128:(tt + 1) * 128], rhs=vb[sb][:, ii, dh * 512:(dh + 1) * 512],
                                                                                 start=(i == 0), stop=(i == 127)), reads=["actT%d" % (i % 2), "vb%d" % sb], writes=["bank%d" % yb])

        def epilogue(S, g):
            for tt in range(2):
                o = ot[tt]
                on = otn[tt]
                r0 = g * BLK + tt * 128
                S.dma(lambda e, o=o, r0=r0: e.dma_start(out=o[:], in_=d["x1s"][r0:r0 + 128, :]), reads=["x1s_%d_%d" % (g, tt)], writes=[on])
                for dh in range(2):
                    yb = tt * 2 + dh
                    S.dve(lambda e, dh=dh, yb=yb: e.tensor_tensor(out=x2[:, dh * 512:(dh + 1) * 512], in0=k.bank[yb][:, :], in1=gt2b[:, dh * 512:(dh + 1) * 512], op=ALU.mult),
                          reads=["bank%d" % yb, "gt2b"], writes=["x2"])
                S.dve(lambda e, o=o: e.tensor_tensor(out=x2[:], in0=x2[:], in1=o[:], op=ALU.add), reads=["x2", on], writes=["x2"])
                S.act(lambda e, o=o: e.activation(out=o[:], in_=x2[:], func=AF.Square, accum_out=ss2[:]), reads=["x2"], writes=[on, "ss2"])
                S.dve(lambda e: e.tensor_scalar(out=ss2[:], in0=ss2[:], scalar1=1.0 / D, scalar2=EPS, op0=ALU.mult, op1=ALU.add), reads=["ss2"], writes=["ss2"])
                S.pool(lambda e: e.tensor_tensor(out=ss2[:], in0=ss2[:], in1=k.neghalf[:, 0:1], op=ALU.pow), reads=["ss2", "neghalf"], writes=["ss2"])
                S.dve(lambda e, o=o: e.scalar_tensor_tensor(out=o[:], in0=x2[:], scalar=ss2[:, 0:1], in1=gfb[:], op0=ALU.mult, op1=ALU.mult), reads=["x2", "ss2", "gfb"], writes=[on])
                S.dma(lambda e, o=o, r0=r0: e.dma_start(out=d["out"][r0:r0 + 128, :], in_=o[:]), reads=[on], writes=["out_%d" % r0])

        ngrp = k.opts.get("ngrp", NBLK)
        prelude(S, 0)
        for g in range(ngrp):
            gcon(S, g)
            rec = Rec()
            if g + 1 < ngrp:
                prelude(rec, g + 1)
            per = (len(rec.l) + 127) // 128
            main_load(S, g, 0)
            main_load(S, g, 1)
            main_A(S, g, 0)
            for i in range(128):
                if (i + 1) % NI == 0 and (i + 1) // NI + 1 < 128 // NI:
                    main_load(S, g, (i + 1) // NI + 1)
                if i + 1 < 128:
                    main_A(S, g, i + 1)
                main_EV(S, g, i)
                rec.replay(S, per)
            rec.replay(S)
            epilogue(S, g)


_CACHE = {}


def make_in_maps(inputs):
    f = lambda a: np.ascontiguousarray(np.asarray(a, dtype=np.float32))
    hc = host_consts()
    common = {
        "w_ada": f(inputs["w_ada"][0]), "b_ada": f(inputs["b_ada"][0]).reshape(1, -1),
        "g1": f(inputs["g_norm1"][0]).reshape(1, -1), "g2": f(inputs["g_norm2"][0]).reshape(1, -1), "gf": f(inputs["g_final"]).reshape(1, -1),
        "w_in": f(inputs["w_in"][0]), "w_au": f(inputs["w_attn_up"][0]), "w_mix": f(inputs["w_pool_mix"][0]),
        "b_mix": f(np.asarray(inputs["b_pool_mix"][0]).T), "pscale": f(np.asarray(inputs["pool_scale"][0]).reshape(4, 128).T),
        "w_pu": f(inputs["w_pool_up"][0]), "w_out": f(inputs["w_out"][0]), "w_pq": f(inputs["w_peer_q"][0]),
        "keys": f(inputs["peer_sub_keys"][0]), "peer_u": f(inputs["peer_u"][0]), "peer_v": f(inputs["peer_v"][0]),
    }
    common.update(hc)
    maps = []
    x = np.asarray(inputs["x"], dtype=np.float32)
    c = np.asarray(inputs["c"], dtype=np.float32)
    for b in range(x.shape[0]):
        m = dict(common)
        m["x"] = np.ascontiguousarray(x[b])
        m["c"] = np.ascontiguousarray(c[b].reshape(8, 128).T)
        maps.append(m)
    return maps


def kernel(**inputs):
    if "nc" not in _CACHE:
        _CACHE["nc"] = build()[0]
    nc = _CACHE["nc"]
    maps = make_in_maps(inputs)
    res = run_bass_kernel_spmd(nc, maps, core_ids=list(range(8)))
    return np.stack([np.asarray(r["out"], dtype=np.float32) for r in res.results], axis=0)
```
